# Optimizing a Trainium2 kernel written in Bass

```python
import jax, jax.numpy as jnp
from jax import lax
import numpy as np

D_MODEL = 1024
BATCH = 4
SEQ = 4096
DEPTH = 1

HEAD_DIM = 64
N_ATTN_HEADS = D_MODEL // (2 * HEAD_DIM)
N_GMLP_GROUPS = D_MODEL // (2 * HEAD_DIM)
ATTN_WIDTH = N_ATTN_HEADS * HEAD_DIM
GMLP_WIDTH = N_GMLP_GROUPS * HEAD_DIM
MIX_WIDTH = ATTN_WIDTH + GMLP_WIDTH
IN_PROJ_WIDTH = 3 * ATTN_WIDTH + 2 * GMLP_WIDTH
MOBA_BLOCK = 256
MOBA_TOPK = 3
Q_CHUNK = 128
GMLP_CHUNK = 128
ROPE_THETA = 10000.0
FFN_HIDDEN = -(-(8 * D_MODEL) // (3 * 256)) * 256
DEEPNORM_ALPHA = (2 * DEPTH) ** 0.25
DEEPNORM_BETA = (8 * DEPTH) ** -0.25
LN_EPS = 1e-5
RMS_EPS = 1e-6
NEG_INF = -1e30

kernel_name = "hymba_moba_gmlp_deepnorm_block"


def layer_norm(x, g, b):
    xf = x.astype(jnp.float32)
    mu = jnp.mean(xf, axis=-1, keepdims=True)
    var = jnp.mean(jnp.square(xf - mu), axis=-1, keepdims=True)
    return ((xf - mu) * lax.rsqrt(var + LN_EPS) * g.astype(jnp.float32) + b.astype(jnp.float32)).astype(x.dtype)


def rms_norm(x, g):
    xf = x.astype(jnp.float32)
    ms = jnp.mean(jnp.square(xf), axis=-1, keepdims=True)
    return (xf * lax.rsqrt(ms + RMS_EPS) * g.astype(jnp.float32)).astype(x.dtype)


def rope(t, pos):
    inv_freq = ROPE_THETA ** (-jnp.arange(0, HEAD_DIM, 2, dtype=jnp.float32) / HEAD_DIM)
    ang = pos.astype(jnp.float32)[:, None] * inv_freq[None, :]
    ang = jnp.concatenate([ang, ang], axis=-1)
    cos = jnp.cos(ang).astype(t.dtype)
    sin = jnp.sin(ang).astype(t.dtype)
    t1, t2 = t[..., : HEAD_DIM // 2], t[..., HEAD_DIM // 2:]
    rot = jnp.concatenate([-t2, t1], axis=-1)
    return t * cos + rot * sin


def moba_attention(q, k, v):
    B, H, S, hd = q.shape
    nb = -(-S // MOBA_BLOCK)
    pad = nb * MOBA_BLOCK - S
    kb = jnp.pad(k, ((0, 0), (0, 0), (0, pad), (0, 0))).reshape(B, H, nb, MOBA_BLOCK, hd)
    vb = jnp.pad(v, ((0, 0), (0, 0), (0, pad), (0, 0))).reshape(B, H, nb, MOBA_BLOCK, hd)
    pos = jnp.arange(S, dtype=jnp.int32)
    own = pos // MOBA_BLOCK
    own_idx = jnp.broadcast_to(own[None, None, :, None], (B, H, S, 1)).astype(jnp.int32)
    n_sel = min(MOBA_TOPK, nb - 1)
    if n_sel > 0:
        k_mean = jnp.mean(kb.astype(jnp.float32), axis=3)
        gate = jnp.einsum('bhsd,bhnd->bhsn', q.astype(jnp.float32), k_mean)
        past = jnp.arange(nb, dtype=jnp.int32)[None, :] < own[:, None]
        gate = jnp.where(past[None, None], gate, NEG_INF)
        _, sel = lax.top_k(gate, n_sel)
        idx = jnp.concatenate([sel.astype(jnp.int32), own_idx], axis=-1)
    else:
        idx = own_idx
    nsb = idx.shape[-1]
    n_chunks = S // Q_CHUNK
    q_c = q.reshape(B, H, n_chunks, Q_CHUNK, hd).transpose(2, 0, 1, 3, 4)
    idx_c = idx.reshape(B, H, n_chunks, Q_CHUNK, nsb).transpose(2, 0, 1, 3, 4)
    pos_c = pos.reshape(n_chunks, Q_CHUNK)
    b_ix = jnp.arange(B)[:, None, None, None]
    h_ix = jnp.arange(H)[None, :, None, None]
    key_off = jnp.arange(MOBA_BLOCK, dtype=jnp.int32)
    slot_own = jnp.arange(nsb) == nsb - 1
    scale = HEAD_DIM ** -0.5

    def attend(args):
        qc, ic, pc = args
        kg = kb[b_ix, h_ix, ic]
        vg = vb[b_ix, h_ix, ic]
        s = jnp.einsum('bhqd,bhqnkd->bhqnk', qc, kg).astype(jnp.float32) * scale
        own_c = pc // MOBA_BLOCK
        valid_blk = slot_own | (ic < own_c[:, None])
        key_pos = ic[..., None] * MOBA_BLOCK + key_off
        mask = valid_blk[..., None] & (key_pos <= pc[:, None, None])
        s = jnp.where(mask, s, NEG_INF)
        p = jax.nn.softmax(s.reshape(B, H, Q_CHUNK, nsb * MOBA_BLOCK), axis=-1)
        p = p.reshape(B, H, Q_CHUNK, nsb, MOBA_BLOCK).astype(vg.dtype)
        return jnp.einsum('bhqnk,bhqnkd->bhqd', p, vg)

    out = lax.map(attend, (q_c, idx_c, pos_c))
    return out.transpose(1, 2, 0, 3, 4).reshape(B, H, S, hd)


def spatial_gating(u, z, ln_g, ln_b, w_s, b_s):
    B, S, G, dg = z.shape
    z = layer_norm(z, ln_g, ln_b)
    nc = S // GMLP_CHUNK
    zc = z.reshape(B, nc, GMLP_CHUNK, G, dg)
    tril = jnp.tril(jnp.ones((GMLP_CHUNK, GMLP_CHUNK), dtype=bool))
    w = jnp.where(tril[None], w_s, jnp.zeros_like(w_s))
    mixed = jnp.einsum('gij,bcjgd->bcigd', w, zc) + b_s.T[None, None, :, :, None]
    return u * mixed.reshape(B, S, G, dg)


def hybrid_mixer(x, w_in, attn_out_g, gmlp_out_g, gmlp_ln_g, gmlp_ln_b, w_spatial, b_spatial, w_out):
    B, S, _ = x.shape
    h = x @ w_in
    A, G = ATTN_WIDTH, GMLP_WIDTH
    q, k, v, u, z = jnp.split(h, [A, 2 * A, 3 * A, 3 * A + G], axis=-1)

    def heads(t):
        return t.reshape(B, S, N_ATTN_HEADS, HEAD_DIM).transpose(0, 2, 1, 3)

    pos = jnp.arange(S, dtype=jnp.int32)
    qh = rope(heads(q), pos)
    kh = rope(heads(k), pos)
    attn = moba_attention(qh, kh, heads(v)).transpose(0, 2, 1, 3).reshape(B, S, A)

    u = jax.nn.gelu(u, approximate=False).reshape(B, S, N_GMLP_GROUPS, HEAD_DIM)
    z = jax.nn.gelu(z, approximate=False).reshape(B, S, N_GMLP_GROUPS, HEAD_DIM)
    sg = spatial_gating(u, z, gmlp_ln_g, gmlp_ln_b, w_spatial, b_spatial).reshape(B, S, G)

    merged = jnp.concatenate([rms_norm(attn, attn_out_g), rms_norm(sg, gmlp_out_g)], axis=-1)
    return merged @ w_out


def swiglu_ffn(x, w_gate, w_up, w_down):
    return (jax.nn.silu(x @ w_gate) * (x @ w_up)) @ w_down


def setup_inputs(seed: int = 0) -> dict:
    key = jax.random.key(seed)
    ks = jax.random.split(key, 16)
    nrm = jax.random.normal
    f32 = jnp.float32
    x = nrm(ks[0], (BATCH, SEQ, D_MODEL), f32)
    w_in = nrm(ks[1], (DEPTH, D_MODEL, IN_PROJ_WIDTH), f32) * D_MODEL ** -0.5
    attn_out_g = 1.0 + 0.1 * nrm(ks[2], (DEPTH, ATTN_WIDTH), f32)
    gmlp_out_g = 1.0 + 0.1 * nrm(ks[3], (DEPTH, GMLP_WIDTH), f32)
    gmlp_ln_g = 1.0 + 0.1 * nrm(ks[4], (DEPTH, N_GMLP_GROUPS, HEAD_DIM), f32)
    gmlp_ln_b = 0.02 * nrm(ks[5], (DEPTH, N_GMLP_GROUPS, HEAD_DIM), f32)
    w_spatial = nrm(ks[6], (DEPTH, N_GMLP_GROUPS, GMLP_CHUNK, GMLP_CHUNK), f32) * GMLP_CHUNK ** -0.5
    b_spatial = 1.0 + 0.1 * nrm(ks[7], (DEPTH, N_GMLP_GROUPS, GMLP_CHUNK), f32)
    w_out = nrm(ks[8], (DEPTH, MIX_WIDTH, D_MODEL), f32) * (MIX_WIDTH ** -0.5 * DEEPNORM_BETA)
    ln1_g = 1.0 + 0.1 * nrm(ks[9], (DEPTH, D_MODEL), f32)
    ln1_b = 0.02 * nrm(ks[10], (DEPTH, D_MODEL), f32)
    w_gate = nrm(ks[11], (DEPTH, D_MODEL, FFN_HIDDEN), f32) * D_MODEL ** -0.5
    w_up = nrm(ks[12], (DEPTH, D_MODEL, FFN_HIDDEN), f32) * D_MODEL ** -0.5
    w_down = nrm(ks[13], (DEPTH, FFN_HIDDEN, D_MODEL), f32) * (FFN_HIDDEN ** -0.5 * DEEPNORM_BETA)
    ln2_g = 1.0 + 0.1 * nrm(ks[14], (DEPTH, D_MODEL), f32)
    ln2_b = 0.02 * nrm(ks[15], (DEPTH, D_MODEL), f32)
    return {"x": x, "w_in": w_in, "attn_out_g": attn_out_g, "gmlp_out_g": gmlp_out_g,
            "gmlp_ln_g": gmlp_ln_g, "gmlp_ln_b": gmlp_ln_b, "w_spatial": w_spatial,
            "b_spatial": b_spatial, "w_out": w_out, "ln1_g": ln1_g, "ln1_b": ln1_b,
            "w_gate": w_gate, "w_up": w_up, "w_down": w_down, "ln2_g": ln2_g, "ln2_b": ln2_b}


def reference(x, w_in, attn_out_g, gmlp_out_g, gmlp_ln_g, gmlp_ln_b, w_spatial, b_spatial,
              w_out, ln1_g, ln1_b, w_gate, w_up, w_down, ln2_g, ln2_b):
    for l in range(DEPTH):
        mix = hybrid_mixer(x, w_in[l], attn_out_g[l], gmlp_out_g[l], gmlp_ln_g[l], gmlp_ln_b[l],
                           w_spatial[l], b_spatial[l], w_out[l])
        x = layer_norm(DEEPNORM_ALPHA * x + mix, ln1_g[l], ln1_b[l])
        ffn = swiglu_ffn(x, w_gate[l], w_up[l], w_down[l])
        x = layer_norm(DEEPNORM_ALPHA * x + ffn, ln2_g[l], ln2_b[l])
    return x
```

```python
import numpy as np
import ml_dtypes
from contextlib import ExitStack
import concourse.bass as bass
import concourse.mybir as mybir
from concourse.bass_utils import run_bass_kernel_spmd

F32 = mybir.dt.float32
BF16 = mybir.dt.bfloat16
AF = mybir.ActivationFunctionType
ALU = mybir.AluOpType
AX = mybir.AxisListType

D = 1024
S = 4096
NT = 32
FF = 2816
NFC = 22
ALPHA = 2.0 ** 0.25
LN_EPS = 1e-5
RMS_EPS = 1e-6
NEG = -30000.0
A_BLK = [0, 3, 4, 7, 8, 11, 12, 15]
B_BLK = [1, 2, 5, 6, 9, 10, 13, 14]


class Tok:
    __slots__ = ("name", "writers", "readers")

    def __init__(self, name=""):
        self.name = name
        self.writers = []
        self.readers = []


class Op:
    __slots__ = ("eng", "fn", "deps", "signal", "sig", "idx", "dma_sem", "dma_val")

    def __init__(self, eng, fn, idx):
        self.eng = eng
        self.fn = fn
        self.deps = []
        self.signal = False
        self.sig = None
        self.idx = idx
        self.dma_sem = None
        self.dma_val = None


class Prog:
    def __init__(self, nc, tag):
        self.nc = nc
        self.tag = tag
        self.ops = []
        self.stream = {"pe": [], "act": [], "dve": [], "pool": [], "sp": []}
        self.dma_list = {}

    def _add_deps(self, op, reads, writes, extra):
        deps = list(extra)
        for t in reads:
            deps.extend(t.writers)
        for t in writes:
            deps.extend(t.readers)
            deps.extend(t.writers)
        latest = {}
        out = []
        for d in deps:
            if d is op:
                continue
            if d.dma_sem is not None:
                if d not in out:
                    out.append(d)
                continue
            if d.eng == "pe" and op.eng == "pe" and op.dma_sem is None:
                continue
            cur = latest.get(d.eng)
            if cur is None or d.idx > cur.idx:
                latest[d.eng] = d
        out.extend(latest.values())
        for d in out:
            d.signal = True
        op.deps = out
        for t in reads:
            self._push(t.readers, op)
        for t in writes:
            if t.readers:
                t.readers = []
                t.writers = [op]
            else:
                self._push(t.writers, op)

    @staticmethod
    def _push(lst, op):
        if op.dma_sem is None:
            for i, o in enumerate(lst):
                if o.dma_sem is None and o.eng == op.eng:
                    lst[i] = op
                    return
        lst.append(op)

    def op(self, eng, fn, reads=(), writes=(), extra=()):
        o = Op(eng, fn, len(self.ops))
        self.ops.append(o)
        self.stream[eng].append(o)
        self._add_deps(o, reads, writes, extra)
        return o

    def dma(self, queue, out, in_, sem_name, reads=(), writes=(), extra=()):
        o = Op(queue, None, len(self.ops))
        o.dma_sem = sem_name
        lst = self.dma_list.setdefault(sem_name, [])
        lst.append(o)
        o.dma_val = 16 * len(lst)
        o.fn = lambda e: e.dma_start(out=out, in_=in_)
        self.ops.append(o)
        self.stream[queue].append(o)
        self._add_deps(o, reads, writes, extra)
        return o

    def last_ops(self):
        return [lst[-1] for k, lst in self.stream.items() if lst and k != "sp"]

    def emit(self):
        nc = self.nc
        for eng, lst in self.stream.items():
            c = 0
            for o in lst:
                if o.dma_sem is None and o.signal:
                    c += 1
                    o.sig = c
        with ExitStack() as es:
            sems = {e: es.enter_context(nc.semaphore(self.tag + "s_" + e)) for e in self.stream}
            dsems = {n: es.enter_context(nc.semaphore(self.tag + "d_" + n)) for n in self.dma_list}
            block = es.enter_context(nc.Block())
            streams = self.stream
            dma_list = self.dma_list

            def run(engname):
                def body(e):
                    waited = {}
                    for o in streams[engname]:
                        for d in o.deps:
                            if d.dma_sem is not None:
                                key = ("d", d.dma_sem)
                                val = 16 * sum(1 for x in dma_list[d.dma_sem] if x.idx < o.idx)
                                sem = dsems[d.dma_sem]
                            else:
                                key = ("c", d.eng)
                                val = d.sig
                                sem = sems[d.eng]
                            if waited.get(key, 0) >= val:
                                continue
                            waited[key] = val
                            e.wait_ge(sem, val)
                        ins = o.fn(e)
                        if o.dma_sem is not None:
                            ins.then_inc(dsems[o.dma_sem], 16)
                        elif o.signal:
                            ins.then_inc(sems[engname], 1)
                    mine = set(o.dma_sem for o in streams[engname] if o.dma_sem is not None)
                    for n in mine:
                        tot = 16 * len(dma_list[n])
                        if waited.get(("d", n), 0) < tot:
                            e.wait_ge(dsems[n], tot)
                return body

            block.tensor(run("pe"))
            block.scalar(run("act"))
            block.vector(run("dve"))
            block.gpsimd(run("pool"))
            block.sync(run("sp"))


def build_program(debug=False):
    nc = bass.Bass("TRN2", target_bir_lowering=False)

    def din(name, shape):
        return nc.dram_tensor(name, list(shape), F32, kind="ExternalInput").ap()

    xs = din("xs", [S, D])
    w_in = din("w_in", [D, 2560])
    w_out = din("w_out", [D, D])
    w_gate = din("w_gate", [D, FF])
    w_up = din("w_up", [D, FF])
    w_down = din("w_down", [FF, D])
    csT_d = din("csT", [128, NT, 96])
    eligb_d = din("eligb", [128, 16, 16])
    elig01_d = din("elig01", [128, 16, 16])
    own01_d = din("own01", [128, 16, 16])
    def din_bf(name, shape):
        return nc.dram_tensor(name, list(shape), BF16, kind="ExternalInput").ap()

    onehot_d = din_bf("onehot", [16, S])
    cmask_d = din_bf("cmask", [128, 2, 256])
    ident_d = din_bf("ident", [128, 128])
    tril_d = din_bf("tril", [128, 128])
    wsT_d = din("wsT", [128, 8, 128])
    bsT_d = din("bsT", [128, 8])
    lng_d = din("lng_b", [128, 512])
    lnb_d = din("lnb_b", [128, 512])
    gog_d = din("gog_b", [128, 512])
    agg_d = din("ag_fm", [128, 4])
    ln1g_d = din("ln1g_b", [128, D])
    ln1b_d = din("ln1b_b", [128, D])
    ln2g_d = din("ln2g_b", [128, D])
    ln2b_d = din("ln2b_b", [128, D])
    out_d = nc.dram_tensor("out", [2048, D], F32, kind="ExternalOutput").ap()

    w_in_v = w_in.rearrange("(kc p) n -> p kc n", p=128)
    wo_s = nc.dram_tensor("wo_s", [8, 128, D], BF16, kind="Internal").ap()
    wg_s = nc.dram_tensor("wg_s", [NFC, 128, 8, 128], BF16, kind="Internal").ap()
    wu_s = nc.dram_tensor("wu_s", [NFC, 128, 8, 128], BF16, kind="Internal").ap()
    wd_s = nc.dram_tensor("wd_s", [NFC, 128, D], BF16, kind="Internal").ap()

    with ExitStack() as es0:
        def sb(name, shape, dt, es=es0):
            return es.enter_context(nc.sbuf_tensor("s_" + name, list(shape), dt))

        def ps(name, shape, dt):
            return es0.enter_context(nc.psum_tensor("p_" + name, list(shape), dt))

        B = [ps("B%d" % i, [128, 512], F32) for i in range(6)]
        B6 = ps("B6", [128, 1024], BF16)
        B7 = ps("B7", [128, 1024], BF16)
        tB = [Tok("B%d" % i) for i in range(6)]
        tB6, tB7 = Tok("B6"), Tok("B7")
        tB7b = tB7

        sgT = sb("sgT", [128, 4, 2048], BF16)
        agT = sb("agT", [128, 4, 2048], BF16)
        ident = sb("ident", [128, 128], BF16)
        ones_bf = sb("ones_bf", [128, 128], BF16)
        epsc = sb("epsc", [128, 4], F32)
        t_sgT, t_agT, t_ident, t_ones = Tok("sgT"), Tok("agT"), Tok("ident"), Tok("ones")
        t_eps = Tok("eps")

        with ExitStack() as es1:
            P = Prog(nc, "a")
            sb1 = lambda n, s, d: sb(n, s, d, es1)
            KT = sb1("KT", [80, 4, S], BF16)
            V2 = sb1("V2", [128, NT, 2, 192], BF16)
            QB = sb1("QB", [128, 16, 4, 80], BF16)
            ov1 = sb1("ov1", [128, 8192], BF16)
            ov2 = sb1("ov2", [128, 4096], BF16)
            QaT = ov2[0:80, :].rearrange("p (a h k) -> p a h k", a=2, h=4)
            BAR = []
            Pbuf = sb1("Pbuf", [128, 4, 512], BF16)
            wqkv = sb1("wqkv", [128, 8, 768], BF16)
            wuz = ov1[:, :].rearrange("p (a b) -> p a b", a=8)
            xb = sb1("xb", [128, 4, D], BF16)
            xT = sb1("xT", [128, 2, 8, 128], BF16)
            cs = sb1("cs", [128, 2, 96], F32)
            eligb = sb1("eligb", [128, 16, 16], F32)
            elig01 = sb1("elig01", [128, 16, 16], F32)
            own01 = sb1("own01", [128, 16, 16], F32)
            cmask = sb1("cmask", [128, 2, 256], BF16)
            tril = sb1("tril", [128, 128], BF16)
            ws_bf = sb1("ws_bf", [128, 8, 128], BF16)
            bsT = sb1("bsT", [128, 8], F32)
            lng = sb1("lng", [128, 512], F32)
            lnb = sb1("lnb", [128, 512], F32)
            gog = sb1("gog", [128, 512], F32)
            agg = sb1("agg", [128, 4], F32)
            qk_f = sb1("qk_f", [128, 1, 512], F32)
            rt1 = sb1("rt1", [128, 512], F32)
            rt2 = sb1("rt2", [128, 512], F32)
            kr_bf = sb1("kr_bf", [128, 2, 256], BF16)
            gu = sb1("gu", [128, 2, 512], F32)
            gz = ov2[:, 0:1024].bitcast(F32)
            cen = ov2[:, 1024:2048].bitcast(F32)
            sq = ov2[:, 2048:3072].bitcast(F32)
            m8 = sb1("m8", [128, 8], F32)
            v8 = sb1("v8", [128, 8], F32)
            zn_bf = sb1("zn_bf", [128, 2, 512], BF16)
            sg_f = ov2[:, 3072:4096].bitcast(F32)
            junk = sb1("junk", [128, 512], BF16)
            ssg = sb1("ssg", [128, 16], F32)
            rg = sb1("rg", [128, 16], F32)
            sgg = sb1("sgg", [128, 2, 512], BF16)
            kms = sb1("kms", [64, 4, 16], F32)
            kms2 = sb1("kms2", [64, NT, 4], F32)
            km_bf = sb1("km_bf", [64, 4, 16], BF16)
            gm = sb1("gm", [128, 16, 16], F32)
            top8 = sb1("top8", [128, 16, 8], F32)
            sel = sb1("sel", [128, 16, 16], F32)
            ssb = ov1[:, 0:4096].bitcast(F32)
            a_pair = ov1[:, 4096:6144].bitcast(F32).rearrange("p (a b) -> p a b", a=2)
            rec = ov1[:, 6144:7168].bitcast(F32)
            sq_bf = ov1[:, 7168:8192].rearrange("p (a b) -> p a b", a=2)
            ra_bf = Pbuf[:, :, :].rearrange("p a b -> p (a b)")

            T = Tok
            t_KT, t_V2, t_QB, t_w, t_wuz, t_tab = T("KT"), T("V2"), T("QB"), T("wqkv"), T("wuz"), T("tab")
            t_oh = T("oh")
            t_cm, t_el, t_agg = T("cm"), T("el"), T("agg")
            t_QaT = [T("QaT0"), T("QaT1")]
            t_P = [T("P%d" % i) for i in range(4)]
            t_xb = [T("xb%d" % i) for i in range(4)]
            t_xT = [T("xT0"), T("xT1")]
            t_qkf = [T("qkf0")]
            t_cs = [T("cs0"), T("cs1")]
            t_rt1, t_rt2 = T("rt1"), T("rt2")
            t_kr = [T("kr0"), T("kr1")]
            t_gu = [T("gu0"), T("gu1")]
            t_gz, t_cen, t_sq, t_m8, t_v8 = T("gz"), T("cen"), T("sq"), T("m8"), T("v8")
            t_zn = [T("zn0"), T("zn1")]
            t_sgf, t_junk, t_ssg, t_rg = T("sgf"), T("junk"), T("ssg"), T("rg")
            t_sgg = [T("sgg0"), T("sgg1")]
            t_ws, t_wsf = T("ws"), T("wsf")
            t_kms, t_km = T("kms"), T("km")
            t_kms2 = T("kms2")
            t_gm, t_top8, t_sel = T("gm"), T("top8"), T("sel")
            t_rec = T("rec")
            t_ap = [T("ap0"), T("ap1")]
            t_sqb = [T("sqb0"), T("sqb1")]
            t_ssb, t_rab = T("ssb"), T("rab")

            kc_ = [0]

            def cdma(queue, out, in_, **kw):
                kc_[0] += 1
                return P.dma(queue, out, in_, "k%d" % kc_[0], **kw)

            cdma("sp", ident[:], ident_d, writes=[t_ident])
            cdma("sp", cmask[:], cmask_d, writes=[t_cm])
            cdma("sp", eligb[:], eligb_d, writes=[t_el])
            cdma("sp", elig01[:], elig01_d, writes=[t_el])
            cdma("sp", own01[:], own01_d, writes=[t_el])
            cdma("sp", tril[:], tril_d, writes=[t_wsf])
            cdma("sp", bsT[:], bsT_d, writes=[t_tab])
            cdma("sp", lng[:], lng_d, writes=[t_tab])
            cdma("sp", lnb[:], lnb_d, writes=[t_tab])
            cdma("sp", gog[:], gog_d, writes=[t_tab])
            cdma("sp", agg[:], agg_d, writes=[t_agg])
            for h in range(4):
                cdma("sp", KT[64:80, h, :], onehot_d, writes=[t_oh])
            P.op("pool", lambda e: e.memset(ones_bf[:], 1.0), writes=[t_ones])
            P.op("pool", lambda e: e.memset(epsc[:, 0:1], LN_EPS), writes=[t_eps])
            P.op("pool", lambda e: e.memset(epsc[:, 1:2], RMS_EPS), writes=[t_eps])
            P.op("pool", lambda e: e.memset(epsc[:, 2:3], LN_EPS / (ALPHA * ALPHA)), writes=[t_eps])
            P.op("pool", lambda e: e.memset(V2[:, :, :, 64:128], 1.0), writes=[t_V2])
            P.op("dve", lambda e: e.memset(ssg[:], 0.0), writes=[t_ssg])

            conv = []
            conv.append((wo_s, w_out.rearrange("(kc p) n -> kc p n", p=128)))
            wg_src = w_gate.rearrange("(kc p) (fc f) -> fc p kc f", p=128, f=128)
            wu_src = w_up.rearrange("(kc p) (fc f) -> fc p kc f", p=128, f=128)
            for fc in range(NFC):
                conv.append((wg_s[fc], wg_src[fc]))
                conv.append((wu_s[fc], wu_src[fc]))
            wd_src = w_down.rearrange("(fc p) n -> fc p n", p=128)
            conv.append((wd_s[0:11], wd_src[0:11]))
            conv.append((wd_s[11:22], wd_src[11:22]))
            conv_it = iter(conv)

            def emit_conv(n):
                for _ in range(n):
                    c = next(conv_it, None)
                    if c is not None:
                        P.dma("pool", c[0], c[1], "cv")

            for hg in range(2):
                for j, base in enumerate((0, 512, 1024)):
                    c0 = base + hg * 256
                    P.dma("pool", wqkv[:, :, j * 256:(j + 1) * 256], w_in_v[:, :, c0:c0 + 256], "w_in",
                          writes=[t_w])
                if hg == 0:
                    P.dma("pool", wuz[:], w_in_v[:, :, 1536:2560], "w_in", writes=[t_wuz])
                    cdma("pool", ws_bf[:], wsT_d, writes=[t_wsf])
                    P.op("dve", lambda e: e.tensor_tensor(
                        ws_bf[:], ws_bf[:], tril[:].unsqueeze(1).to_broadcast([128, 8, 128]), ALU.mult),
                        reads=[t_wsf], writes=[t_ws, t_wsf])

                def xload(t):
                    P.dma("pool", xb[:, t % 4, :], xs[t * 128:(t + 1) * 128, :], "x%d" % (t % 4),
                          writes=[t_xb[t % 4]])

                def stageX(t, hg=hg):
                    s = t % 2
                    s4 = t % 4
                    if t + 2 < NT:
                        xload(t + 2)
                    for kc in range(8):
                        P.op("pe", lambda e, kc=kc: e.transpose(
                            B6[:, kc * 128:(kc + 1) * 128], xb[:, s4, kc * 128:(kc + 1) * 128], ident[:]),
                            reads=[t_xb[s4], t_ident], writes=[tB6])
                    P.op("act", lambda e: e.copy(xT[:, s, :, :].rearrange("p a b -> p (a b)"), B6[:]),
                         reads=[tB6], writes=[t_xT[s]])

                def stageA(t, hg=hg):
                    own = t < 16
                    s = t % 2
                    do_g = own and hg == 0
                    rope_eng = "pool" if (hg == 0 and own) else "dve"
                    P.dma("sp", cs[:, s, :], csT_d[:, t, :], "cs%d" % s, writes=[t_cs[s]])
                    c_lo = 0 if own else 256
                    for kc in range(8):
                        P.op("pe", lambda e, kc=kc: e.matmul(
                            B[0][:, c_lo:512], xT[:, s, kc, :], wqkv[:, kc, c_lo:512],
                            start=(kc == 0), stop=(kc == 7)),
                            reads=[t_xT[s], t_w], writes=[tB[0]])
                    for kc in range(8):
                        P.op("pe", lambda e, kc=kc: e.matmul(
                            B[1][:, 0:256], xT[:, s, kc, :], wqkv[:, kc, 512:768],
                            start=(kc == 0), stop=(kc == 7)),
                            reads=[t_xT[s], t_w], writes=[tB[1]])
                    if do_g:
                        for kc in range(8):
                            P.op("pe", lambda e, kc=kc: e.matmul(
                                B[2][:], xT[:, s, kc, :], wuz[:, kc, 0:512],
                                start=(kc == 0), stop=(kc == 7)),
                                reads=[t_xT[s], t_wuz], writes=[tB[2]])
                        for kc in range(8):
                            P.op("pe", lambda e, kc=kc: e.matmul(
                                B[3][:], xT[:, s, kc, :], wuz[:, kc, 512:1024],
                                start=(kc == 0), stop=(kc == 7)),
                                reads=[t_xT[s], t_wuz], writes=[tB[3]])
                    P.op("act", lambda e: e.copy(qk_f[:, 0, c_lo:512], B[0][:, c_lo:512]),
                         reads=[tB[0]], writes=[t_qkf[0]])
                    P.op("act", lambda e: e.copy(
                        V2[:, t, :, :].rearrange("p a (e c) -> p a e c", c=64)[:, :, 0:3:2, :],
                        B[1][:, 0:256].rearrange("p (a e c) -> p a e c", e=2, c=64)),
                        reads=[tB[1]], writes=[t_V2])
                    if do_g:
                        P.op("act", lambda e: e.activation(gu[:, s, :], B[2][:], AF.Gelu),
                             reads=[tB[2]], writes=[t_gu[s]])
                        P.op("act", lambda e: e.activation(gz[:], B[3][:], AF.Gelu),
                             reads=[tB[3]], writes=[t_gz])
                    nh = 8 if own else 4
                    qv = qk_f[:, 0, c_lo:512].rearrange("p (h d) -> p h d", d=64)
                    r2 = rt2[:, c_lo:512].rearrange("p (h d) -> p h d", d=64)
                    qv4 = qk_f[:, 0, c_lo:512].rearrange("p (h a d) -> p h a d", a=2, d=32)
                    r14 = rt1[:, c_lo:512].rearrange("p (h a d) -> p h a d", a=2, d=32)
                    cb = cs[:, s, 0:32].unsqueeze(1).unsqueeze(1).to_broadcast([128, nh, 2, 32])
                    sb_lo = cs[:, s, 32:64].unsqueeze(1).to_broadcast([128, nh, 32])
                    sb_hi = cs[:, s, 64:96].unsqueeze(1).to_broadcast([128, nh, 32])
                    P.op(rope_eng, lambda e: e.tensor_tensor(r14, qv4, cb, ALU.mult),
                         reads=[t_qkf[0], t_cs[s]], writes=[t_rt1])
                    P.op(rope_eng, lambda e: e.tensor_tensor(r2[:, :, 0:32], qv[:, :, 32:64], sb_lo, ALU.mult),
                         reads=[t_qkf[0], t_cs[s]], writes=[t_rt2])
                    P.op(rope_eng, lambda e: e.tensor_tensor(r2[:, :, 32:64], qv[:, :, 0:32], sb_hi, ALU.mult),
                         reads=[t_qkf[0], t_cs[s]], writes=[t_rt2])
                    if own:
                        P.op(rope_eng, lambda e: e.tensor_tensor(
                            QB[:, t, :, 0:64], rt1[:, 0:256].rearrange("p (h d) -> p h d", d=64),
                            rt2[:, 0:256].rearrange("p (h d) -> p h d", d=64), ALU.add),
                            reads=[t_rt1, t_rt2], writes=[t_QB])
                    P.op(rope_eng, lambda e: e.tensor_tensor(
                        kr_bf[:, s, :], rt1[:, 256:512], rt2[:, 256:512], ALU.add),
                        reads=[t_rt1, t_rt2], writes=[t_kr[s]])
                def stageA2(t, hg=hg):
                    own = t < 16
                    s = t % 2
                    do_g = own and hg == 0
                    if do_g:
                        gz3 = gz[:].rearrange("p (g d) -> p g d", d=64)
                        cen3 = cen[:].rearrange("p (g d) -> p g d", d=64)
                        sq3 = sq[:].rearrange("p (g d) -> p g d", d=64)
                        P.op("dve", lambda e: e.tensor_reduce(m8[:], gz3, AX.X, ALU.add),
                             reads=[t_gz], writes=[t_m8])
                        P.op("dve", lambda e: e.scalar_tensor_tensor(
                            cen3, m8[:].unsqueeze(2).to_broadcast([128, 8, 64]), -1.0 / 64.0, gz3,
                            ALU.mult, ALU.add),
                            reads=[t_gz, t_m8], writes=[t_cen])
                        P.op("dve", lambda e: e.tensor_tensor(sq[:], cen[:], cen[:], ALU.mult),
                             reads=[t_cen], writes=[t_sq])
                        P.op("dve", lambda e: e.tensor_reduce(v8[:], sq3, AX.X, ALU.add),
                             reads=[t_sq], writes=[t_v8])

                def stageA2b(t, hg=hg):
                    own = t < 16
                    s = t % 2
                    do_g = own and hg == 0
                    if do_g:
                        cen3 = cen[:].rearrange("p (g d) -> p g d", d=64)
                        sq3 = sq[:].rearrange("p (g d) -> p g d", d=64)
                        P.op("act", lambda e: e.activation(v8[:], v8[:], AF.Sqrt, scale=1.0 / 64.0, bias=epsc[:, 0:1]),
                             reads=[t_v8, t_eps], writes=[t_v8])
                        P.op("dve", lambda e: e.reciprocal(v8[:], v8[:]),
                             reads=[t_v8], writes=[t_v8])
                        P.op("dve", lambda e: e.tensor_tensor(
                            sq3, cen3, v8[:].unsqueeze(2).to_broadcast([128, 8, 64]), ALU.mult),
                            reads=[t_cen, t_v8], writes=[t_sq])
                        P.op("dve", lambda e: e.tensor_tensor(cen[:], sq[:], lng[:], ALU.mult),
                             reads=[t_sq, t_tab], writes=[t_cen])
                        P.op("dve", lambda e: e.tensor_tensor(zn_bf[:, s, :], cen[:], lnb[:], ALU.add),
                             reads=[t_cen, t_tab], writes=[t_zn[s]])

                def stageB(t, hg=hg):
                    own = t < 16
                    s = t % 2
                    do_g = own and hg == 0
                    for h in range(4):
                        P.op("pe", lambda e, h=h: e.transpose(
                            B7[0:64, h * 128:(h + 1) * 128], kr_bf[:, s, h * 64:(h + 1) * 64], ident[:]),
                            reads=[t_kr[s], t_ident], writes=[tB7])
                    P.op("dve", lambda e: e.tensor_copy(
                        KT[0:64, :, t * 128:(t + 1) * 128],
                        B7[0:64, 0:512].rearrange("p (h k) -> p h k", k=128)),
                        reads=[tB7], writes=[t_KT])
                    P.op("dve", lambda e: e.tensor_reduce(
                        kms2[:, t, :], B7[0:64, 0:512].rearrange("p (h k) -> p h k", k=128), AX.X, ALU.add),
                        reads=[tB7], writes=[t_kms2])
                    if do_g:
                        for g in range(8):
                            P.op("pe", lambda e, g=g: e.matmul(
                                B[4][:, g * 64:(g + 1) * 64], ws_bf[:, g, :], zn_bf[:, s, g * 64:(g + 1) * 64],
                                start=True, stop=True),
                                reads=[t_zn[s], t_ws], writes=[tB[4]])
                        P.op("dve", lambda e: e.tensor_tensor(
                            sg_f[:].rearrange("p (g d) -> p g d", d=64),
                            B[4][:].rearrange("p (g d) -> p g d", d=64),
                            bsT[:].unsqueeze(2).to_broadcast([128, 8, 64]), ALU.add),
                            reads=[tB[4], t_tab], writes=[t_sgf])
                        P.op("dve", lambda e: e.tensor_tensor(sg_f[:], sg_f[:], gu[:, s, :], ALU.mult),
                             reads=[t_sgf, t_gu[s]], writes=[t_sgf])
                        P.op("act", lambda e: e.activation(junk[:], sg_f[:], AF.Square,
                                                           accum_out=ssg[:, t:t + 1]),
                             reads=[t_sgf], writes=[t_junk, t_ssg])

                def stageB1b(t, hg=hg):
                    own = t < 16
                    s = t % 2
                    do_g = own and hg == 0
                    if do_g:
                        P.op("act", lambda e: e.activation(
                            rg[:, t:t + 1], ssg[:, t:t + 1], AF.Sqrt, scale=1.0 / 512.0, bias=epsc[:, 1:2]),
                            reads=[t_ssg, t_eps], writes=[t_rg])
                        P.op("dve", lambda e: e.reciprocal(rg[:, t:t + 1], rg[:, t:t + 1]),
                             reads=[t_rg], writes=[t_rg])
                        P.op("dve", lambda e: e.scalar_tensor_tensor(
                            sgg[:, s, :], sg_f[:], rg[:, t:t + 1], gog[:], ALU.mult, ALU.mult),
                            reads=[t_sgf, t_rg, t_tab], writes=[t_sgg[s]])

                def stageB2(t, hg=hg):
                    own = t < 16
                    s = t % 2
                    do_g = own and hg == 0
                    if do_g:
                        for c in range(4):
                            P.op("pe", lambda e, c=c: e.transpose(
                                B7[:, 512 + c * 128:512 + (c + 1) * 128], sgg[:, s, c * 128:(c + 1) * 128],
                                ident[:]),
                                reads=[t_sgg[s], t_ident], writes=[tB7b])
                        P.op("act", lambda e: e.copy(
                            sgT[:, :, t * 128:(t + 1) * 128],
                            B7[:, 512:1024].rearrange("p (c k) -> p c k", k=128)),
                            reads=[tB7b], writes=[t_sgT])

                for t in range(2):
                    xload(t)
                stageX(0)
                stageX(1)
                stageA(0)
                stageA2(0)
                stageA2b(0)
                for t in range(NT):
                    if t + 2 < NT:
                        stageX(t + 2)
                    if t + 1 < NT:
                        stageA(t + 1)
                    stageB(t)
                    if t + 1 < NT:
                        stageA2(t + 1)
                    stageB1b(t)
                    if t >= 1:
                        stageB2(t - 1)
                    if t + 1 < NT:
                        stageA2b(t + 1)
                stageB2(NT - 1)

                if hg == 0:
                    BAR.extend(P.last_ops())
                kv = kms2[:].rearrange("p (n two) h -> p n two h", two=2)
                P.op("dve", lambda e: e.tensor_tensor(
                    kms[:].rearrange("p h n -> p n h"), kv[:, :, 0, :], kv[:, :, 1, :], ALU.add),
                    reads=[t_kms2], writes=[t_kms])
                P.op("dve", lambda e: e.tensor_scalar(km_bf[:], kms[:], 1.0 / 256.0, None, ALU.mult),
                     reads=[t_kms], writes=[t_km])

                rot = {"s": 0, "p": 0}
                deferred = []

                def gate_prep(Tq, hg=hg):
                    qs = Tq % 2
                    for half, bank, tb in ((0, B6, [tB6]), (1, B7, [tB7])):
                        for jj in range(2):
                            j = half * 2 + jj
                            tt = Tq * 4 + j
                            for h in range(4):
                                P.op("pe", lambda e, bank=bank, jj=jj, h=h, tt=tt: e.transpose(
                                    bank[0:64, (jj * 4 + h) * 128:(jj * 4 + h + 1) * 128],
                                    QB[:, tt, h, 0:64], ident[:]),
                                    reads=[t_QB, t_ident], writes=tb)
                        P.op("act", lambda e, bank=bank, half=half: e.copy(
                            QaT[0:64, qs, :, half * 256:(half + 1) * 256].rearrange("p h (j k) -> p j h k", k=128),
                            bank[0:64, :].rearrange("p (j h k) -> p j h k", h=4, k=128)),
                            reads=tb, writes=[t_QaT[qs]], extra=BAR)
                    for j in range(4):
                        for h in range(4):
                            P.op("pe", lambda e, j=j, h=h: e.matmul(
                                B[5][:, (j * 4 + h) * 16:(j * 4 + h + 1) * 16],
                                QaT[0:64, qs, h, j * 128:(j + 1) * 128], km_bf[:, h, :], start=True, stop=True),
                                reads=[t_QaT[qs], t_km], writes=[tB[5]])
                    el_b = eligb[:, Tq * 4:Tq * 4 + 4, :].unsqueeze(2).to_broadcast([128, 4, 4, 16])
                    el_1 = elig01[:, Tq * 4:Tq * 4 + 4, :].unsqueeze(2).to_broadcast([128, 4, 4, 16])
                    ow_1 = own01[:, Tq * 4:Tq * 4 + 4, :].unsqueeze(2).to_broadcast([128, 4, 4, 16])
                    gm4 = gm[:].rearrange("p (j h) n -> p j h n", h=4)
                    sel4 = sel[:].rearrange("p (j h) n -> p j h n", h=4)
                    P.op("dve", lambda e: e.tensor_tensor(
                        gm4, B[5][:, 0:256].rearrange("p (j h n) -> p j h n", h=4, n=16), el_b, ALU.add),
                        reads=[tB[5], t_el], writes=[t_gm])
                    for jh in range(16):
                        P.op("dve", lambda e, jh=jh: e.max(top8[:, jh, :], gm[:, jh, :]),
                             reads=[t_gm], writes=[t_top8])
                    P.op("dve", lambda e: e.tensor_tensor(
                        sel[:], gm[:], top8[:, :, 2:3].to_broadcast([128, 16, 16]), ALU.is_ge),
                        reads=[t_gm, t_top8], writes=[t_sel])
                    P.op("dve", lambda e: e.tensor_tensor(sel4, sel4, el_1, ALU.mult),
                         reads=[t_sel, t_el], writes=[t_sel])
                    P.op("dve", lambda e: e.tensor_tensor(sel4, sel4, ow_1, ALU.max),
                         reads=[t_sel, t_el], writes=[t_sel])
                    P.op("dve", lambda e: e.tensor_scalar(
                        QB[:, Tq * 4:Tq * 4 + 4, :, 64:80], sel4, -1.0, -NEG, ALU.add, ALU.mult),
                        reads=[t_sel], writes=[t_QB])

                def qaug(Tq):
                    qs = Tq % 2
                    for half, bank, tb in ((0, B6, [tB6]), (1, B7, [tB7])):
                        for jj in range(2):
                            j = half * 2 + jj
                            tt = Tq * 4 + j
                            for h in range(4):
                                P.op("pe", lambda e, bank=bank, jj=jj, h=h, tt=tt: e.transpose(
                                    bank[0:80, (jj * 4 + h) * 128:(jj * 4 + h + 1) * 128],
                                    QB[:, tt, h, :], ident[:]),
                                    reads=[t_QB, t_ident], writes=tb)
                        P.op("act", lambda e, bank=bank, half=half: e.copy(
                            QaT[:, qs, :, half * 256:(half + 1) * 256].rearrange("p h (j k) -> p j h k", k=128),
                            bank[0:80, :].rearrange("p (j h k) -> p j h k", h=4, k=128)),
                            reads=tb, writes=[t_QaT[qs]], extra=BAR)

                mid_deferred = []

                def flush_mid():
                    for fn in mid_deferred:
                        fn()
                    del mid_deferred[:]

                def flush_deferred():
                    flush_mid()
                    for fn in deferred:
                        fn()
                    del deferred[:]

                def attn_head(Tq, h, hg=hg):
                    qs = Tq % 2
                    klist = []
                    for kb in list(range(0, 2 * Tq + 2)) + list(range(8, 8 + 2 * Tq + 2)):
                        c0 = 256 if kb in (2 * Tq + 1, 8 + 2 * Tq + 1) else 0
                        diag = kb in (2 * Tq, 2 * Tq + 1)
                        for kt in range(2):
                            klist.append((kb, kt, c0, diag))
                    pr, ee = h // 2, h % 2
                    ob = 3 + (h % 2)
                    num0 = 0 if ee == 0 else 64
                    den0 = 64 if ee == 0 else 0
                    nk = len(klist)

                    def emit_qk(i):
                        kb, kt, c0, diag = klist[i]
                        sbk = rot["s"] % 3
                        rot["s"] += 1
                        ktile = kb * 2 + kt
                        P.op("pe", lambda e: e.matmul(
                            B[sbk][:, c0:512], KT[0:80, h, ktile * 128:(ktile + 1) * 128],
                            QaT[:, qs, h, c0:512], start=True, stop=(not diag)),
                            reads=[t_KT, t_oh, t_QaT[qs]], writes=[tB[sbk]])
                        if diag:
                            d0 = 0 if kb == 2 * Tq else 256
                            P.op("pe", lambda e: e.matmul(
                                B[sbk][:, d0:d0 + 256], ident[:], cmask[:, kt, :], start=False, stop=True),
                                reads=[t_ident, t_cm], writes=[tB[sbk]])
                        return sbk

                    def emit_rest(i, sbk):
                        kb, kt, c0, diag = klist[i]
                        pb = rot["p"] % 4
                        rot["p"] += 1
                        ktile = kb * 2 + kt
                        st_, sp_ = (i == 0), (i == nk - 1)
                        P.op("act", lambda e: e.activation(
                            Pbuf[:, pb, c0:512], B[sbk][:, c0:512], AF.Exp, scale=0.125),
                            reads=[tB[sbk]], writes=[t_P[pb]])
                        P.op("pe", lambda e: e.matmul(
                            B[ob][:, c0:512], V2[:, ktile, pr, ee * 64:ee * 64 + 128], Pbuf[:, pb, c0:512],
                            start=st_, stop=sp_),
                            reads=[t_V2, t_P[pb]], writes=[tB[ob]])

                    def run(pre, next_qk):
                        sbl = list(pre)
                        nxt = []
                        for i in range(nk):
                            if i + 2 < nk:
                                sbl.append(emit_qk(i + 2))
                            elif next_qk is not None:
                                nxt.append(next_qk(i + 2 - nk))
                            emit_rest(i, sbl[i])
                            if i == 7:
                                flush_mid()
                            if hg == 0 and i % 6 == 3:
                                emit_conv(1)
                        P.op("dve", lambda e: e.reciprocal(
                            rec[num0:num0 + 64, :], B[ob][den0:den0 + 64, :]),
                            reads=[tB[ob]], writes=[t_rec], extra=BAR)
                        P.op("dve", lambda e: e.tensor_tensor(
                            a_pair[num0:num0 + 64, pr, :], B[ob][num0:num0 + 64, :], rec[num0:num0 + 64, :],
                            ALU.mult),
                            reads=[tB[ob], t_rec], writes=[t_ap[pr]])
                        if ee == 1:
                            gp = hg * 2 + pr

                            def pair_act():
                                P.op("dve", lambda e: e.tensor_tensor(
                                    sq_bf[:, pr, :], a_pair[:, pr, :], a_pair[:, pr, :], ALU.mult),
                                    reads=[t_ap[pr]], writes=[t_sqb[pr]], extra=BAR)
                                P.op("dve", lambda e: e.tensor_scalar(
                                    agT[:, gp, Tq * 512:(Tq + 1) * 512], a_pair[:, pr, :], agg[:, gp:gp + 1], None,
                                    ALU.mult),
                                    reads=[t_ap[pr], t_agg], writes=[t_agT])
                            mid_deferred.append(pair_act)

                            def ssmm(pr=pr):
                                P.op("pe", lambda e: e.matmul(
                                    B[5][:], ones_bf[:], sq_bf[:, pr, :], start=(pr == 0), stop=(pr == 1)),
                                    reads=[t_ones, t_sqb[pr]], writes=[tB[5]])
                            deferred.append(ssmm)
                            if pr == 1:
                                def ssacc(Tq=Tq):
                                    if hg == 0:
                                        P.op("dve", lambda e: e.tensor_copy(ssb[:, Tq * 512:(Tq + 1) * 512], B[5][:]),
                                             reads=[tB[5]], writes=[t_ssb], extra=BAR)
                                    else:
                                        P.op("dve", lambda e: e.tensor_tensor(
                                            ssb[:, Tq * 512:(Tq + 1) * 512], ssb[:, Tq * 512:(Tq + 1) * 512], B[5][:],
                                            ALU.add),
                                            reads=[tB[5], t_ssb], writes=[t_ssb])
                                deferred.append(ssacc)
                        return nxt
                    return emit_qk, run

                gate_prep(0)
                qaug(0)
                for Tq in range(4):
                    heads = [attn_head(Tq, h) for h in range(4)]
                    pre = [heads[0][0](0), heads[0][0](1)]
                    for h in range(4):
                        if h == 3 and Tq + 1 < 4:
                            gate_prep(Tq + 1)
                        pre = heads[h][1](pre, heads[h + 1][0] if h + 1 < 4 else None)
                        if h == 0:
                            flush_deferred()
                    if Tq + 1 < 4:
                        qaug(Tq + 1)
                flush_deferred()
                if hg == 0:
                    emit_conv(100)


            P.op("act", lambda e: e.activation(ssb[:], ssb[:], AF.Ln, scale=1.0 / 512.0, bias=epsc[:, 1:2]),
                 reads=[t_ssb, t_eps], writes=[t_ssb])
            P.op("act", lambda e: e.activation(ra_bf, ssb[:], AF.Exp, scale=-0.5),
                 reads=[t_ssb], writes=[t_rab] + t_P)
            for gp in range(4):
                P.op("dve", lambda e, gp=gp: e.tensor_tensor(agT[:, gp, :], agT[:, gp, :], ra_bf, ALU.mult),
                     reads=[t_rab, t_agT], writes=[t_agT])
            P.emit()
        nc.all_engine_barrier()
        for tk in tB + [tB6, tB7, t_sgT, t_agT, t_ident, t_ones, t_eps]:
            tk.readers = []
            tk.writers = []

        with ExitStack() as es2:
            P = Prog(nc, "b")
            sb2 = lambda n, s, d: sb(n, s, d, es2)
            wo = sb2("wo", [128, 8, D], BF16)
            wd = sb2("wd", [128, NFC, D], BF16)
            wg = sb2("wg", [128, 3, 8, 128], BF16)
            wu = sb2("wu", [128, 3, 8, 128], BF16)
            hT = sb2("hT", [128, NFC, 512], BF16)
            x1T = sb2("x1T", [128, 8, 512], BF16)
            xin = sb2("xin", [128, 1, D], F32)
            y = sb2("y", [128, 4, D], F32)
            x1 = sb2("x1", [128, 4, D], F32)
            x1b = sb2("x1b", [128, 4, D], BF16)
            st1 = sb2("st1", [128, 4, 16], F32)
            st2 = sb2("st2", [128, 4, 16], F32)
            sl = sb2("sl", [128, 1, 512], F32)
            ln1g = sb2("ln1g", [128, D], F32)
            ln1b = sb2("ln1b", [128, D], F32)
            ln2g = sb2("ln2g", [128, D], F32)
            ln2b = sb2("ln2b", [128, D], F32)
            y2 = y
            T = Tok
            t_wo, t_wd, t_tab2 = T("wo"), T("wd"), T("tab2")
            t_tab2b = T("tab2b")
            t_wgu = [T("wgu%d" % i) for i in range(3)]
            t_hT, t_x1T = T("hT"), T("x1T")
            t_xin = [T("xin0"), T("xin1")]
            t_y = [T("y%d" % i) for i in range(4)]
            t_x1 = [T("x1_%d" % i) for i in range(4)]
            t_x1b = [T("x1b%d" % i) for i in range(4)]
            t_st1 = T("st1")
            t_st2 = [T("st2_%d" % i) for i in range(4)]
            t_sl = [T("sl0")] * 2
            t_y2 = t_y

            for j in range(4):
                P.dma("sp", y[:, j, :], xs[j * 128:(j + 1) * 128, :], "xg0_%d" % j, writes=[t_y[j]])
            P.dma("sp", wo[:], wo_s.rearrange("kc p n -> p kc n"), "wo", writes=[t_wo])
            P.dma("sp", ln1g[:], ln1g_d, "c2a", writes=[t_tab2])
            P.dma("sp", ln1b[:], ln1b_d, "c2b", writes=[t_tab2])
            P.dma("sp", ln2g[:], ln2g_d, "c2c", writes=[t_tab2b])
            P.dma("sp", ln2b[:], ln2b_d, "c2d", writes=[t_tab2b])
            wd_v = wd_s.rearrange("fc p n -> p fc n")
            EPS1 = LN_EPS / (ALPHA * ALPHA)

            def ln_chain(views, vtoks, stt, t_stt, cols, g_t, b_t, post):
                for v, tk, c in zip(views, vtoks, cols):
                    P.op("dve", lambda e, v=v, c=c: e.bn_stats(stt[:, c, 0:6], v[:, 0:512]),
                         reads=[tk], writes=[t_stt])
                    P.op("dve", lambda e, v=v, c=c: e.bn_stats(stt[:, c, 6:12], v[:, 512:1024]),
                         reads=[tk], writes=[t_stt])
                    P.op("dve", lambda e, c=c: e.bn_aggr(stt[:, c, 12:14], stt[:, c, 0:12]),
                         reads=[t_stt], writes=[t_stt])
                return lambda: ln_part2(views, vtoks, stt, t_stt, cols, g_t, b_t, post)

            def ln_part2(views, vtoks, stt, t_stt, cols, g_t, b_t, post):
                c0, c1 = cols[0], cols[-1] + 1
                P.op("act", lambda e: e.activation(stt[:, c0:c1, 14], stt[:, c0:c1, 13], AF.Sqrt, bias=epsc[:, 2:3]),
                     reads=[t_stt, t_eps], writes=[t_stt])
                P.op("dve", lambda e: e.reciprocal(stt[:, c0:c1, 14], stt[:, c0:c1, 14]),
                     reads=[t_stt], writes=[t_stt])
                P.op("dve", lambda e: e.scalar_tensor_tensor(
                    stt[:, c0:c1, 15], stt[:, c0:c1, 12], -1.0, stt[:, c0:c1, 14], ALU.mult, ALU.mult),
                    reads=[t_stt], writes=[t_stt])
                for i, (v, tk, c) in enumerate(zip(views, vtoks, cols)):
                    P.op("act", lambda e, v=v, c=c: e.activation(
                        v, v, AF.Identity, scale=stt[:, c, 14:15], bias=stt[:, c, 15:16]),
                        reads=[t_stt, tk], writes=[tk])
                    P.op("dve", lambda e, v=v: e.tensor_tensor(v, v, g_t[:], ALU.mult),
                         reads=[tk, t_tab2], writes=[tk])
                    P.op("dve", lambda e, v=v: e.tensor_tensor(v, v, b_t[:], ALU.add),
                         reads=[tk, t_tab2], writes=[tk])
                    post(i)

            def W_mm(g, j):
                tt = g * 4 + j
                s = tt % 2
                if g == 0:
                    xres, t_xres = y[:, j, :], t_y[j]
                else:
                    P.dma("sp", xin[:, 0, :], xs[tt * 128:(tt + 1) * 128, :], "xin0", writes=[t_xin[0]])
                    xres, t_xres = xin[:, 0, :], t_xin[0]
                for hf in range(2):
                    bk = s * 2 + hf
                    for kc in range(8):
                        lhs = agT[:, kc, tt * 128:(tt + 1) * 128] if kc < 4 else sgT[:, kc - 4, tt * 128:(tt + 1) * 128]
                        P.op("pe", lambda e, lhs=lhs, bk=bk, kc=kc, hf=hf: e.matmul(
                            B[bk][:], lhs, wo[:, kc, hf * 512:(hf + 1) * 512], start=(kc == 0), stop=(kc == 7)),
                            reads=[t_agT, t_sgT, t_wo], writes=[tB[bk]])
                    P.op("dve", lambda e, bk=bk, hf=hf: e.scalar_tensor_tensor(
                        x1[:, j, hf * 512:(hf + 1) * 512], B[bk][:], 1.0 / ALPHA, xres[:, hf * 512:(hf + 1) * 512],
                        ALU.mult, ALU.add),
                        reads=[tB[bk], t_xres], writes=[t_x1[j]])

            def W_chain(g, js):
                def post(i):
                    j = js[i]
                    P.op("act", lambda e: e.copy(x1b[:, j, :], x1[:, j, :]),
                         reads=[t_x1[j]], writes=[t_x1b[j]])
                return ln_chain([x1[:, j, :] for j in js], [t_x1[j] for j in js], st1, t_st1, list(js),
                                ln1g, ln1b, post)

            def W_tr(g, j):
                bank, tbk = (B6, tB6) if j % 2 == 0 else (B7, tB7)
                for kc in range(8):
                    P.op("pe", lambda e, kc=kc: e.transpose(
                        bank[:, kc * 128:(kc + 1) * 128], x1b[:, j, kc * 128:(kc + 1) * 128], ident[:]),
                        reads=[t_x1b[j], t_ident], writes=[tbk])
                P.op("act", lambda e: e.copy(
                    x1T[:, :, j * 128:(j + 1) * 128], bank[:].rearrange("p (c k) -> p c k", k=128)),
                    reads=[tbk], writes=[t_x1T])

            def G_stage(g, steps=()):
                steps = list(steps)
                for fc in range(NFC):
                    ws_ = (g * NFC + fc) % 3
                    P.dma("sp", wg[:, ws_, :, :], wg_s[fc], "wgu%d" % ws_, writes=[t_wgu[ws_]])
                    P.dma("sp", wu[:, ws_, :, :], wu_s[fc], "wgu%d" % ws_, writes=[t_wgu[ws_]])
                    if g == 0 and 2 <= fc < 13:
                        i = fc - 2
                        P.dma("sp", wd[:, i * 2:(i + 1) * 2, :], wd_v[:, i * 2:(i + 1) * 2, :], "wd",
                              writes=[t_wd])
                    for kc in range(8):
                        P.op("pe", lambda e, kc=kc, ws_=ws_: e.matmul(
                            B[4][:], wg[:, ws_, kc, :], x1T[:, kc, :], start=(kc == 0), stop=(kc == 7)),
                            reads=[t_wgu[ws_], t_x1T], writes=[tB[4]])
                    for kc in range(8):
                        P.op("pe", lambda e, kc=kc, ws_=ws_: e.matmul(
                            B[5][:], wu[:, ws_, kc, :], x1T[:, kc, :], start=(kc == 0), stop=(kc == 7)),
                            reads=[t_wgu[ws_], t_x1T], writes=[tB[5]])
                    P.op("act", lambda e: e.activation(sl[:, 0, :], B[4][:], AF.Silu),
                         reads=[tB[4]], writes=[t_sl[0]])
                    P.op("dve", lambda e, fc=fc: e.tensor_tensor(hT[:, fc, :], sl[:, 0, :], B[5][:], ALU.mult),
                         reads=[tB[5], t_sl[0]], writes=[t_hT])
                    for _ in range(2):
                        if steps:
                            steps.pop(0)()
                while steps:
                    steps.pop(0)()

            def Dn(g, j):
                tt = g * 4 + j
                s = tt % 2
                for hf in range(2):
                    bk = s * 2 + hf
                    for fc in range(NFC):
                        P.op("pe", lambda e, bk=bk, fc=fc, hf=hf: e.matmul(
                            B[bk][:], hT[:, fc, j * 128:(j + 1) * 128], wd[:, fc, hf * 512:(hf + 1) * 512],
                            start=(fc == 0), stop=(fc == NFC - 1)),
                            reads=[t_hT, t_wd], writes=[tB[bk]])
                    P.op("dve", lambda e, bk=bk, hf=hf: e.scalar_tensor_tensor(
                        y[:, j, hf * 512:(hf + 1) * 512], B[bk][:], 1.0 / ALPHA, x1[:, j, hf * 512:(hf + 1) * 512],
                        ALU.mult, ALU.add),
                        reads=[tB[bk], t_x1[j]], writes=[t_y[j]])
                v, tk, stt, tst, c = y[:, j, :], t_y[j], st2, t_st2[j], j
                steps = []
                steps.append(lambda: P.op("dve", lambda e: e.bn_stats(stt[:, c, 0:6], v[:, 0:512]),
                                          reads=[tk], writes=[tst]))
                steps.append(lambda: (
                    P.op("dve", lambda e: e.bn_stats(stt[:, c, 6:12], v[:, 512:1024]), reads=[tk], writes=[tst]),
                    P.op("dve", lambda e: e.bn_aggr(stt[:, c, 12:14], stt[:, c, 0:12]), reads=[tst], writes=[tst])))
                steps.append(lambda: P.op(
                    "act", lambda e: e.activation(stt[:, c, 14:15], stt[:, c, 13:14], AF.Sqrt, bias=epsc[:, 2:3]),
                    reads=[tst, t_eps], writes=[tst]))
                steps.append(lambda: (
                    P.op("dve", lambda e: e.reciprocal(stt[:, c, 14:15], stt[:, c, 14:15]), reads=[tst], writes=[tst]),
                    P.op("dve", lambda e: e.scalar_tensor_tensor(
                        stt[:, c, 15:16], stt[:, c, 12:13], -1.0, stt[:, c, 14:15], ALU.mult, ALU.mult),
                        reads=[tst], writes=[tst])))
                steps.append(lambda: P.op(
                    "act", lambda e: e.activation(v, v, AF.Identity, scale=stt[:, c, 14:15], bias=stt[:, c, 15:16]),
                    reads=[tst, tk], writes=[tk]))
                steps.append(lambda: P.op("dve", lambda e: e.tensor_tensor(v, v, ln2g[:], ALU.mult),
                                          reads=[tk, t_tab2b], writes=[tk]))
                steps.append(lambda: (
                    P.op("dve", lambda e: e.tensor_tensor(v, v, ln2b[:], ALU.add), reads=[tk, t_tab2b], writes=[tk]),
                    P.dma("pool", out_d[tt * 128:(tt + 1) * 128, :], v, "out", reads=[tk])))
                return steps

            def round_robin(step_lists):
                out = []
                n = max(len(x) for x in step_lists)
                for i in range(n):
                    for x in step_lists:
                        if i < len(x):
                            out.append(x[i])
                return out

            for _ in range(24):
                P.op("pe", lambda e: e.matmul(B[5][:], ident[:], agT[:, 0, 0:512], start=True, stop=True),
                     reads=[t_ident, t_agT], writes=[tB[5]])
            W_mm(0, 0)
            c0 = W_chain(0, (0,))
            W_mm(0, 1)
            c0()
            c1 = W_chain(0, (1,))
            W_mm(0, 2)
            c1()
            c2 = W_chain(0, (2,))
            W_tr(0, 0)
            W_mm(0, 3)
            c2()
            c3 = W_chain(0, (3,))
            W_tr(0, 1)
            c3()
            W_tr(0, 2)
            W_tr(0, 3)
            pend = []
            for g in range(4):
                G_stage(g, pend)
                pend = []
                d0 = Dn(g, 0)
                d1 = Dn(g, 1)
                if g + 1 < 4:
                    W_mm(g + 1, 0)
                    W_mm(g + 1, 1)
                    cA = W_chain(g + 1, (0, 1))
                    d2 = Dn(g, 2)
                    cA()
                    W_mm(g + 1, 2)
                    c2 = W_chain(g + 1, (2,))
                    d3 = Dn(g, 3)
                    W_tr(g + 1, 0)
                    W_tr(g + 1, 1)
                    c2()
                    W_mm(g + 1, 3)
                    c3 = W_chain(g + 1, (3,))
                    W_tr(g + 1, 2)
                    c3()
                    W_tr(g + 1, 3)
                    pend = round_robin([d0, d1, d2, d3])
                else:
                    d2 = Dn(g, 2)
                    for st_ in round_robin([d0, d1]):
                        st_()
                    d3 = Dn(g, 3)
                    for st_ in round_robin([d2, d3]):
                        st_()
            P.emit()
    return nc


def _host_tables(e):
    own = A_BLK if e == 0 else B_BLK
    oth = B_BLK if e == 0 else A_BLK
    blocks = own + oth
    tok_idx = np.concatenate([np.arange(g * 256, (g + 1) * 256) for g in blocks])
    inv_freq = (10000.0 ** (-np.arange(0, 64, 2, dtype=np.float32) / 64.0)).astype(np.float32)
    ang = tok_idx.astype(np.float32)[:, None] * inv_freq[None, :]
    cos = np.cos(ang).astype(np.float32)
    sin = np.sin(ang).astype(np.float32)
    cs = np.concatenate([cos, -sin, sin], axis=1)
    csT = np.ascontiguousarray(cs.reshape(NT, 128, 96).transpose(1, 0, 2))
    eligb = np.zeros((16, 16), np.float32)
    elig01 = np.zeros((16, 16), np.float32)
    own01 = np.zeros((16, 16), np.float32)
    for tt in range(16):
        i = tt // 2
        gi = own[i]
        for kb in range(16):
            el = blocks[kb] < gi
            eligb[tt, kb] = 0.0 if el else -1e30
            elig01[tt, kb] = 1.0 if el else 0.0
            own01[tt, kb] = 1.0 if kb == i else 0.0
    bc = lambda a: np.ascontiguousarray(np.broadcast_to(a[None], (128,) + a.shape))
    return tok_idx, csT, bc(eligb), bc(elig01), bc(own01)


def _const_tables():
    onehot = np.zeros((16, S), np.float32)
    for kb in range(16):
        onehot[kb, kb * 256:(kb + 1) * 256] = 1.0
    cmask = np.zeros((128, 2, 256), np.float32)
    p = np.arange(128)[:, None]
    qq = np.arange(256)[None, :]
    for kt in range(2):
        cmask[:, kt, :] = np.where(kt * 128 + p <= qq, 0.0, NEG)
    ident = np.eye(128, dtype=np.float32)
    j = np.arange(128)[:, None]
    i = np.arange(128)[None, :]
    tril = (j <= i).astype(np.float32)
    bf = ml_dtypes.bfloat16
    return onehot.astype(bf), cmask.astype(bf), ident.astype(bf), tril.astype(bf)


_NC_CACHE = {}


def kernel(x, w_in, attn_out_g, gmlp_out_g, gmlp_ln_g, gmlp_ln_b, w_spatial, b_spatial,
           w_out, ln1_g, ln1_b, w_gate, w_up, w_down, ln2_g, ln2_b):
    f = lambda a: np.ascontiguousarray(np.asarray(a, dtype=np.float32))
    x = f(x)
    bc = lambda v, n: np.ascontiguousarray(np.broadcast_to(f(v).reshape(1, n), (128, n)))
    onehot, cmask, ident, tril = _const_tables()
    shared = {
        "w_in": f(w_in[0]), "w_out": f(w_out[0]), "w_gate": f(w_gate[0]), "w_up": f(w_up[0]),
        "w_down": f(w_down[0]),
        "onehot": onehot, "cmask": cmask, "ident": ident, "tril": tril,
        "wsT": np.ascontiguousarray(f(w_spatial[0]).transpose(2, 0, 1)),
        "bsT": np.ascontiguousarray(f(b_spatial[0]).T),
        "lng_b": bc(gmlp_ln_g[0], 512), "lnb_b": bc(gmlp_ln_b[0], 512), "gog_b": bc(gmlp_out_g[0], 512),
        "ag_fm": np.ascontiguousarray(f(attn_out_g[0]).reshape(4, 128).T),
        "ln1g_b": bc(ln1_g[0], D), "ln1b_b": bc(ln1_b[0], D),
        "ln2g_b": bc(ln2_g[0], D), "ln2b_b": bc(ln2_b[0], D),
    }
    in_maps = []
    idxs = []
    for c in range(8):
        b, e = c // 2, c % 2
        tok_idx, csT, eligb, elig01, own01 = _host_tables(e)
        idxs.append(tok_idx)
        m = dict(shared)
        m.update({"xs": np.ascontiguousarray(x[b][tok_idx]), "csT": csT,
                  "eligb": eligb, "elig01": elig01, "own01": own01})
        in_maps.append(m)
    if "nc" not in _NC_CACHE:
        _NC_CACHE["nc"] = build_program()
    nc = _NC_CACHE["nc"]
    res = run_bass_kernel_spmd(nc, in_maps, core_ids=list(range(8)))
    out = np.empty((4, S, D), np.float32)
    for c in range(8):
        b = c // 2
        out[b][idxs[c][:2048]] = res.results[c]["out"]
    return out
```

```python
import numpy as np
import ml_dtypes
from contextlib import ExitStack
import concourse.bass as bass
import concourse.mybir as mybir
from concourse.bass_utils import run_bass_kernel_spmd

F32 = mybir.dt.float32
BF16 = mybir.dt.bfloat16
AF = mybir.ActivationFunctionType
ALU = mybir.AluOpType
AX = mybir.AxisListType

D = 1024
S = 4096
NT = 32
FF = 2816
NFC = 22
ALPHA = 2.0 ** 0.25
LN_EPS = 1e-5
RMS_EPS = 1e-6
NEG = -30000.0
A_BLK = [0, 3, 4, 7, 8, 11, 12, 15]
B_BLK = [1, 2, 5, 6, 9, 10, 13, 14]


class Tok:
    __slots__ = ("name", "writers", "readers")

    def __init__(self, name=""):
        self.name = name
        self.writers = []
        self.readers = []


class Op:
    __slots__ = ("eng", "fn", "deps", "signal", "sig", "idx", "dma_sem", "dma_val")

    def __init__(self, eng, fn, idx):
        self.eng = eng
        self.fn = fn
        self.deps = []
        self.signal = False
        self.sig = None
        self.idx = idx
        self.dma_sem = None
        self.dma_val = None


class Prog:
    def __init__(self, nc, tag):
        self.nc = nc
        self.tag = tag
        self.ops = []
        self.stream = {"pe": [], "act": [], "dve": [], "pool": [], "sp": []}
        self.dma_list = {}

    def _add_deps(self, op, reads, writes, extra):
        deps = list(extra)
        for t in reads:
            deps.extend(t.writers)
        for t in writes:
            deps.extend(t.readers)
            deps.extend(t.writers)
        latest = {}
        out = []
        for d in deps:
            if d is op:
                continue
            if d.dma_sem is not None:
                if d not in out:
                    out.append(d)
                continue
            if d.eng == "pe" and op.eng == "pe" and op.dma_sem is None:
                continue
            cur = latest.get(d.eng)
            if cur is None or d.idx > cur.idx:
                latest[d.eng] = d
        out.extend(latest.values())
        for d in out:
            d.signal = True
        op.deps = out
        for t in reads:
            self._push(t.readers, op)
        for t in writes:
            if t.readers:
                t.readers = []
                t.writers = [op]
            else:
                self._push(t.writers, op)

    @staticmethod
    def _push(lst, op):
        if op.dma_sem is None:
            for i, o in enumerate(lst):
                if o.dma_sem is None and o.eng == op.eng:
                    lst[i] = op
                    return
        lst.append(op)

    def op(self, eng, fn, reads=(), writes=(), extra=()):
        o = Op(eng, fn, len(self.ops))
        self.ops.append(o)
        self.stream[eng].append(o)
        self._add_deps(o, reads, writes, extra)
        return o

    def dma(self, queue, out, in_, sem_name, reads=(), writes=(), extra=()):
        o = Op(queue, None, len(self.ops))
        o.dma_sem = sem_name
        lst = self.dma_list.setdefault(sem_name, [])
        lst.append(o)
        o.dma_val = 16 * len(lst)
        o.fn = lambda e: e.dma_start(out=out, in_=in_)
        self.ops.append(o)
        self.stream[queue].append(o)
        self._add_deps(o, reads, writes, extra)
        return o

    def last_ops(self):
        return [lst[-1] for k, lst in self.stream.items() if lst and k != "sp"]

    def emit(self):
        nc = self.nc
        for eng, lst in self.stream.items():
            c = 0
            for o in lst:
                if o.dma_sem is None and o.signal:
                    c += 1
                    o.sig = c
        with ExitStack() as es:
            sems = {e: es.enter_context(nc.semaphore(self.tag + "s_" + e)) for e in self.stream}
            dsems = {n: es.enter_context(nc.semaphore(self.tag + "d_" + n)) for n in self.dma_list}
            block = es.enter_context(nc.Block())
            streams = self.stream
            dma_list = self.dma_list

            def run(engname):
                def body(e):
                    waited = {}
                    for o in streams[engname]:
                        for d in o.deps:
                            if d.dma_sem is not None:
                                key = ("d", d.dma_sem)
                                val = 16 * sum(1 for x in dma_list[d.dma_sem] if x.idx < o.idx)
                                sem = dsems[d.dma_sem]
                            else:
                                key = ("c", d.eng)
                                val = d.sig
                                sem = sems[d.eng]
                            if waited.get(key, 0) >= val:
                                continue
                            waited[key] = val
                            e.wait_ge(sem, val)
                        ins = o.fn(e)
                        if o.dma_sem is not None:
                            ins.then_inc(dsems[o.dma_sem], 16)
                        elif o.signal:
                            ins.then_inc(sems[engname], 1)
                    mine = set(o.dma_sem for o in streams[engname] if o.dma_sem is not None)
                    for n in mine:
                        tot = 16 * len(dma_list[n])
                        if waited.get(("d", n), 0) < tot:
                            e.wait_ge(dsems[n], tot)
                return body

            block.tensor(run("pe"))
            block.scalar(run("act"))
            block.vector(run("dve"))
            block.gpsimd(run("pool"))
            block.sync(run("sp"))


def build_program(debug=False):
    nc = bass.Bass("TRN2", target_bir_lowering=False)

    def din(name, shape):
        return nc.dram_tensor(name, list(shape), F32, kind="ExternalInput").ap()

    xs = din("xs", [S, D])
    w_in = din("w_in", [D, 2560])
    w_out = din("w_out", [D, D])
    w_gate = din("w_gate", [D, FF])
    w_up = din("w_up", [D, FF])
    w_down = din("w_down", [FF, D])
    csT_d = din("csT", [128, NT, 96])
    eligb_d = din("eligb", [128, 16, 16])
    elig01_d = din("elig01", [128, 16, 16])
    own01_d = din("own01", [128, 16, 16])
    def din_bf(name, shape):
        return nc.dram_tensor(name, list(shape), BF16, kind="ExternalInput").ap()

    onehot_d = din_bf("onehot", [16, S])
    cmask_d = din_bf("cmask", [128, 2, 256])
    ident_d = din_bf("ident", [128, 128])
    tril_d = din_bf("tril", [128, 128])
    wsT_d = din("wsT", [128, 8, 128])
    bsT_d = din("bsT", [128, 8])
    lng_d = din("lng_b", [128, 512])
    lnb_d = din("lnb_b", [128, 512])
    gog_d = din("gog_b", [128, 512])
    agg_d = din("ag_fm", [128, 4])
    ln1g_d = din("ln1g_b", [128, D])
    ln1b_d = din("ln1b_b", [128, D])
    ln2g_d = din("ln2g_b", [128, D])
    ln2b_d = din("ln2b_b", [128, D])
    out_d = nc.dram_tensor("out", [2048, D], F32, kind="ExternalOutput").ap()

    w_in_v = w_in.rearrange("(kc p) n -> p kc n", p=128)
    wo_s = nc.dram_tensor("wo_s", [8, 128, D], BF16, kind="Internal").ap()
    wg_s = nc.dram_tensor("wg_s", [NFC, 128, 8, 128], BF16, kind="Internal").ap()
    wu_s = nc.dram_tensor("wu_s", [NFC, 128, 8, 128], BF16, kind="Internal").ap()
    wd_s = nc.dram_tensor("wd_s", [NFC, 128, D], BF16, kind="Internal").ap()

    with ExitStack() as es0:
        def sb(name, shape, dt, es=es0):
            return es.enter_context(nc.sbuf_tensor("s_" + name, list(shape), dt))

        def ps(name, shape, dt):
            return es0.enter_context(nc.psum_tensor("p_" + name, list(shape), dt))

        B = [ps("B%d" % i, [128, 512], F32) for i in range(6)]
        B6 = ps("B6", [128, 1024], BF16)
        B7 = ps("B7", [128, 1024], BF16)
        tB = [Tok("B%d" % i) for i in range(6)]
        tB6, tB7 = Tok("B6"), Tok("B7")
        tB7b = tB7

        sgT = sb("sgT", [128, 4, 2048], BF16)
        agT = sb("agT", [128, 4, 2048], BF16)
        ident = sb("ident", [128, 128], BF16)
        ones_bf = sb("ones_bf", [128, 128], BF16)
        epsc = sb("epsc", [128, 4], F32)
        t_sgT, t_agT, t_ident, t_ones = Tok("sgT"), Tok("agT"), Tok("ident"), Tok("ones")
        t_eps = Tok("eps")

        with ExitStack() as es1:
            P = Prog(nc, "a")
            sb1 = lambda n, s, d: sb(n, s, d, es1)
            KT = sb1("KT", [80, 4, S], BF16)
            V2 = sb1("V2", [128, NT, 2, 192], BF16)
            QB = sb1("QB", [128, 16, 4, 80], BF16)
            ov1 = sb1("ov1", [128, 8192], BF16)
            ov2 = sb1("ov2", [128, 4096], BF16)
            QaT = ov2[0:80, :].rearrange("p (a h k) -> p a h k", a=2, h=4)
            BAR = []
            Pbuf = sb1("Pbuf", [128, 4, 512], BF16)
            wqkv = sb1("wqkv", [128, 8, 768], BF16)
            wuz = ov1[:, :].rearrange("p (a b) -> p a b", a=8)
            xb = sb1("xb", [128, 4, D], BF16)
            xT = sb1("xT", [128, 2, 8, 128], BF16)
            cs = sb1("cs", [128, 2, 96], F32)
            eligb = sb1("eligb", [128, 16, 16], F32)
            elig01 = sb1("elig01", [128, 16, 16], F32)
            own01 = sb1("own01", [128, 16, 16], F32)
            cmask = sb1("cmask", [128, 2, 256], BF16)
            tril = sb1("tril", [128, 128], BF16)
            ws_bf = sb1("ws_bf", [128, 8, 128], BF16)
            bsT = sb1("bsT", [128, 8], F32)
            lng = sb1("lng", [128, 512], F32)
            lnb = sb1("lnb", [128, 512], F32)
            gog = sb1("gog", [128, 512], F32)
            agg = sb1("agg", [128, 4], F32)
            qk_f = sb1("qk_f", [128, 1, 512], F32)
            rt1 = sb1("rt1", [128, 512], F32)
            rt2 = sb1("rt2", [128, 512], F32)
            kr_bf = sb1("kr_bf", [128, 2, 256], BF16)
            gu = sb1("gu", [128, 2, 512], F32)
            gz = ov2[:, 0:1024].bitcast(F32)
            cen = ov2[:, 1024:2048].bitcast(F32)
            sq = ov2[:, 2048:3072].bitcast(F32)
            m8 = sb1("m8", [128, 8], F32)
            v8 = sb1("v8", [128, 8], F32)
            zn_bf = sb1("zn_bf", [128, 2, 512], BF16)
            sg_f = ov2[:, 3072:4096].bitcast(F32)
            junk = sb1("junk", [128, 512], BF16)
            ssg = sb1("ssg", [128, 16], F32)
            rg = sb1("rg", [128, 16], F32)
            sgg = sb1("sgg", [128, 2, 512], BF16)
            kms = sb1("kms", [64, 4, 16], F32)
            kms2 = sb1("kms2", [64, NT, 4], F32)
            km_bf = sb1("km_bf", [64, 4, 16], BF16)
            gm = sb1("gm", [128, 16, 16], F32)
            top8 = sb1("top8", [128, 16, 8], F32)
            sel = sb1("sel", [128, 16, 16], F32)
            ssb = ov1[:, 0:4096].bitcast(F32)
            a_pair = ov1[:, 4096:6144].bitcast(F32).rearrange("p (a b) -> p a b", a=2)
            rec = ov1[:, 6144:7168].bitcast(F32)
            sq_bf = ov1[:, 7168:8192].rearrange("p (a b) -> p a b", a=2)
            ra_bf = Pbuf[:, :, :].rearrange("p a b -> p (a b)")

            T = Tok
            t_KT, t_V2, t_QB, t_w, t_wuz, t_tab = T("KT"), T("V2"), T("QB"), T("wqkv"), T("wuz"), T("tab")
            t_oh = T("oh")
            t_cm, t_el, t_agg = T("cm"), T("el"), T("agg")
            t_QaT = [T("QaT0"), T("QaT1")]
            t_P = [T("P%d" % i) for i in range(4)]
            t_xb = [T("xb%d" % i) for i in range(4)]
            t_xT = [T("xT0"), T("xT1")]
            t_qkf = [T("qkf0")]
            t_cs = [T("cs0"), T("cs1")]
            t_rt1, t_rt2 = T("rt1"), T("rt2")
            t_kr = [T("kr0"), T("kr1")]
            t_gu = [T("gu0"), T("gu1")]
            t_gz, t_cen, t_sq, t_m8, t_v8 = T("gz"), T("cen"), T("sq"), T("m8"), T("v8")
            t_zn = [T("zn0"), T("zn1")]
            t_sgf, t_junk, t_ssg, t_rg = T("sgf"), T("junk"), T("ssg"), T("rg")
            t_sgg = [T("sgg0"), T("sgg1")]
            t_ws, t_wsf = T("ws"), T("wsf")
            t_kms, t_km = T("kms"), T("km")
            t_kms2 = T("kms2")
            t_gm, t_top8, t_sel = T("gm"), T("top8"), T("sel")
            t_rec = T("rec")
            t_ap = [T("ap0"), T("ap1")]
            t_sqb = [T("sqb0"), T("sqb1")]
            t_ssb, t_rab = T("ssb"), T("rab")

            kc_ = [0]

            def cdma(queue, out, in_, **kw):
                kc_[0] += 1
                return P.dma(queue, out, in_, "k%d" % kc_[0], **kw)

            cdma("sp", ident[:], ident_d, writes=[t_ident])
            cdma("sp", cmask[:], cmask_d, writes=[t_cm])
            cdma("sp", eligb[:], eligb_d, writes=[t_el])
            cdma("sp", elig01[:], elig01_d, writes=[t_el])
            cdma("sp", own01[:], own01_d, writes=[t_el])
            cdma("sp", tril[:], tril_d, writes=[t_wsf])
            cdma("sp", bsT[:], bsT_d, writes=[t_tab])
            cdma("sp", lng[:], lng_d, writes=[t_tab])
            cdma("sp", lnb[:], lnb_d, writes=[t_tab])
            cdma("sp", gog[:], gog_d, writes=[t_tab])
            cdma("sp", agg[:], agg_d, writes=[t_agg])
            for h in range(4):
                cdma("sp", KT[64:80, h, :], onehot_d, writes=[t_oh])
            P.op("pool", lambda e: e.memset(ones_bf[:], 1.0), writes=[t_ones])
            P.op("pool", lambda e: e.memset(epsc[:, 0:1], LN_EPS), writes=[t_eps])
            P.op("pool", lambda e: e.memset(epsc[:, 1:2], RMS_EPS), writes=[t_eps])
            P.op("pool", lambda e: e.memset(epsc[:, 2:3], LN_EPS / (ALPHA * ALPHA)), writes=[t_eps])
            P.op("pool", lambda e: e.memset(V2[:, :, :, 64:128], 1.0), writes=[t_V2])
            P.op("dve", lambda e: e.memset(ssg[:], 0.0), writes=[t_ssg])

            conv = []
            conv.append((wo_s, w_out.rearrange("(kc p) n -> kc p n", p=128)))
            wg_src = w_gate.rearrange("(kc p) (fc f) -> fc p kc f", p=128, f=128)
            wu_src = w_up.rearrange("(kc p) (fc f) -> fc p kc f", p=128, f=128)
            for fc in range(NFC):
                conv.append((wg_s[fc], wg_src[fc]))
                conv.append((wu_s[fc], wu_src[fc]))
            wd_src = w_down.rearrange("(fc p) n -> fc p n", p=128)
            conv.append((wd_s[0:11], wd_src[0:11]))
            conv.append((wd_s[11:22], wd_src[11:22]))
            conv_it = iter(conv)

            def emit_conv(n):
                for _ in range(n):
                    c = next(conv_it, None)
                    if c is not None:
                        P.dma("pool", c[0], c[1], "cv")

            for hg in range(2):
                for j, base in enumerate((0, 512, 1024)):
                    c0 = base + hg * 256
                    P.dma("pool", wqkv[:, :, j * 256:(j + 1) * 256], w_in_v[:, :, c0:c0 + 256], "w_in",
                          writes=[t_w])
                if hg == 0:
                    P.dma("pool", wuz[:], w_in_v[:, :, 1536:2560], "w_in", writes=[t_wuz])
                    cdma("pool", ws_bf[:], wsT_d, writes=[t_wsf])
                    P.op("dve", lambda e: e.tensor_tensor(
                        ws_bf[:], ws_bf[:], tril[:].unsqueeze(1).to_broadcast([128, 8, 128]), ALU.mult),
                        reads=[t_wsf], writes=[t_ws, t_wsf])

                def xload(t):
                    P.dma("pool", xb[:, t % 4, :], xs[t * 128:(t + 1) * 128, :], "x%d" % (t % 4),
                          writes=[t_xb[t % 4]])

                def stageX(t, hg=hg):
                    s = t % 2
                    s4 = t % 4
                    if t + 2 < NT:
                        xload(t + 2)
                    for kc in range(8):
                        P.op("pe", lambda e, kc=kc: e.transpose(
                            B6[:, kc * 128:(kc + 1) * 128], xb[:, s4, kc * 128:(kc + 1) * 128], ident[:]),
                            reads=[t_xb[s4], t_ident], writes=[tB6])
                    P.op("act", lambda e: e.copy(xT[:, s, :, :].rearrange("p a b -> p (a b)"), B6[:]),
                         reads=[tB6], writes=[t_xT[s]])

                def stageA(t, hg=hg):
                    own = t < 16
                    s = t % 2
                    do_g = own and hg == 0
                    rope_eng = "pool" if (hg == 0 and own) else "dve"
                    P.dma("sp", cs[:, s, :], csT_d[:, t, :], "cs%d" % s, writes=[t_cs[s]])
                    c_lo = 0 if own else 256
                    for kc in range(8):
                        P.op("pe", lambda e, kc=kc: e.matmul(
                            B[0][:, c_lo:512], xT[:, s, kc, :], wqkv[:, kc, c_lo:512],
                            start=(kc == 0), stop=(kc == 7)),
                            reads=[t_xT[s], t_w], writes=[tB[0]])
                    for kc in range(8):
                        P.op("pe", lambda e, kc=kc: e.matmul(
                            B[1][:, 0:256], xT[:, s, kc, :], wqkv[:, kc, 512:768],
                            start=(kc == 0), stop=(kc == 7)),
                            reads=[t_xT[s], t_w], writes=[tB[1]])
                    if do_g:
                        for kc in range(8):
                            P.op("pe", lambda e, kc=kc: e.matmul(
                                B[2][:], xT[:, s, kc, :], wuz[:, kc, 0:512],
                                start=(kc == 0), stop=(kc == 7)),
                                reads=[t_xT[s], t_wuz], writes=[tB[2]])
                        for kc in range(8):
                            P.op("pe", lambda e, kc=kc: e.matmul(
                                B[3][:], xT[:, s, kc, :], wuz[:, kc, 512:1024],
                                start=(kc == 0), stop=(kc == 7)),
                                reads=[t_xT[s], t_wuz], writes=[tB[3]])
                    P.op("act", lambda e: e.copy(qk_f[:, 0, c_lo:512], B[0][:, c_lo:512]),
                         reads=[tB[0]], writes=[t_qkf[0]])
                    P.op("act", lambda e: e.copy(
                        V2[:, t, :, :].rearrange("p a (e c) -> p a e c", c=64)[:, :, 0:3:2, :],
                        B[1][:, 0:256].rearrange("p (a e c) -> p a e c", e=2, c=64)),
                        reads=[tB[1]], writes=[t_V2])
                    if do_g:
                        P.op("act", lambda e: e.activation(gu[:, s, :], B[2][:], AF.Gelu),
                             reads=[tB[2]], writes=[t_gu[s]])
                        P.op("act", lambda e: e.activation(gz[:], B[3][:], AF.Gelu),
                             reads=[tB[3]], writes=[t_gz])
                    nh = 8 if own else 4
                    qv = qk_f[:, 0, c_lo:512].rearrange("p (h d) -> p h d", d=64)
                    r2 = rt2[:, c_lo:512].rearrange("p (h d) -> p h d", d=64)
                    qv4 = qk_f[:, 0, c_lo:512].rearrange("p (h a d) -> p h a d", a=2, d=32)
                    r14 = rt1[:, c_lo:512].rearrange("p (h a d) -> p h a d", a=2, d=32)
                    cb = cs[:, s, 0:32].unsqueeze(1).unsqueeze(1).to_broadcast([128, nh, 2, 32])
                    sb_lo = cs[:, s, 32:64].unsqueeze(1).to_broadcast([128, nh, 32])
                    sb_hi = cs[:, s, 64:96].unsqueeze(1).to_broadcast([128, nh, 32])
                    P.op(rope_eng, lambda e: e.tensor_tensor(r14, qv4, cb, ALU.mult),
                         reads=[t_qkf[0], t_cs[s]], writes=[t_rt1])
                    P.op(rope_eng, lambda e: e.tensor_tensor(r2[:, :, 0:32], qv[:, :, 32:64], sb_lo, ALU.mult),
                         reads=[t_qkf[0], t_cs[s]], writes=[t_rt2])
                    P.op(rope_eng, lambda e: e.tensor_tensor(r2[:, :, 32:64], qv[:, :, 0:32], sb_hi, ALU.mult),
                         reads=[t_qkf[0], t_cs[s]], writes=[t_rt2])
                    if own:
                        P.op(rope_eng, lambda e: e.tensor_tensor(
                            QB[:, t, :, 0:64], rt1[:, 0:256].rearrange("p (h d) -> p h d", d=64),
                            rt2[:, 0:256].rearrange("p (h d) -> p h d", d=64), ALU.add),
                            reads=[t_rt1, t_rt2], writes=[t_QB])
                    P.op(rope_eng, lambda e: e.tensor_tensor(
                        kr_bf[:, s, :], rt1[:, 256:512], rt2[:, 256:512], ALU.add),
                        reads=[t_rt1, t_rt2], writes=[t_kr[s]])
                def stageA2(t, hg=hg):
                    own = t < 16
                    s = t % 2
                    do_g = own and hg == 0
                    if do_g:
                        gz3 = gz[:].rearrange("p (g d) -> p g d", d=64)
                        cen3 = cen[:].rearrange("p (g d) -> p g d", d=64)
                        sq3 = sq[:].rearrange("p (g d) -> p g d", d=64)
                        P.op("dve", lambda e: e.tensor_reduce(m8[:], gz3, AX.X, ALU.add),
                             reads=[t_gz], writes=[t_m8])
                        P.op("dve", lambda e: e.scalar_tensor_tensor(
                            cen3, m8[:].unsqueeze(2).to_broadcast([128, 8, 64]), -1.0 / 64.0, gz3,
                            ALU.mult, ALU.add),
                            reads=[t_gz, t_m8], writes=[t_cen])
                        P.op("dve", lambda e: e.tensor_tensor(sq[:], cen[:], cen[:], ALU.mult),
                             reads=[t_cen], writes=[t_sq])
                        P.op("dve", lambda e: e.tensor_reduce(v8[:], sq3, AX.X, ALU.add),
                             reads=[t_sq], writes=[t_v8])

                def stageA2b(t, hg=hg):
                    own = t < 16
                    s = t % 2
                    do_g = own and hg == 0
                    if do_g:
                        cen3 = cen[:].rearrange("p (g d) -> p g d", d=64)
                        sq3 = sq[:].rearrange("p (g d) -> p g d", d=64)
                        P.op("act", lambda e: e.activation(v8[:], v8[:], AF.Sqrt, scale=1.0 / 64.0, bias=epsc[:, 0:1]),
                             reads=[t_v8, t_eps], writes=[t_v8])
                        P.op("dve", lambda e: e.reciprocal(v8[:], v8[:]),
                             reads=[t_v8], writes=[t_v8])
                        P.op("dve", lambda e: e.tensor_tensor(
                            sq3, cen3, v8[:].unsqueeze(2).to_broadcast([128, 8, 64]), ALU.mult),
                            reads=[t_cen, t_v8], writes=[t_sq])
                        P.op("dve", lambda e: e.tensor_tensor(cen[:], sq[:], lng[:], ALU.mult),
                             reads=[t_sq, t_tab], writes=[t_cen])
                        P.op("dve", lambda e: e.tensor_tensor(zn_bf[:, s, :], cen[:], lnb[:], ALU.add),
                             reads=[t_cen, t_tab], writes=[t_zn[s]])

                def stageB(t, hg=hg):
                    own = t < 16
                    s = t % 2
                    do_g = own and hg == 0
                    for h in range(4):
                        P.op("pe", lambda e, h=h: e.transpose(
                            B7[0:64, h * 128:(h + 1) * 128], kr_bf[:, s, h * 64:(h + 1) * 64], ident[:]),
                            reads=[t_kr[s], t_ident], writes=[tB7])
                    P.op("dve", lambda e: e.tensor_copy(
                        KT[0:64, :, t * 128:(t + 1) * 128],
                        B7[0:64, 0:512].rearrange("p (h k) -> p h k", k=128)),
                        reads=[tB7], writes=[t_KT])
                    P.op("dve", lambda e: e.tensor_reduce(
                        kms2[:, t, :], B7[0:64, 0:512].rearrange("p (h k) -> p h k", k=128), AX.X, ALU.add),
                        reads=[tB7], writes=[t_kms2])
                    if do_g:
                        for g in range(8):
                            P.op("pe", lambda e, g=g: e.matmul(
                                B[4][:, g * 64:(g + 1) * 64], ws_bf[:, g, :], zn_bf[:, s, g * 64:(g + 1) * 64],
                                start=True, stop=True),
                                reads=[t_zn[s], t_ws], writes=[tB[4]])
                        P.op("dve", lambda e: e.tensor_tensor(
                            sg_f[:].rearrange("p (g d) -> p g d", d=64),
                            B[4][:].rearrange("p (g d) -> p g d", d=64),
                            bsT[:].unsqueeze(2).to_broadcast([128, 8, 64]), ALU.add),
                            reads=[tB[4], t_tab], writes=[t_sgf])
                        P.op("dve", lambda e: e.tensor_tensor(sg_f[:], sg_f[:], gu[:, s, :], ALU.mult),
                             reads=[t_sgf, t_gu[s]], writes=[t_sgf])
                        P.op("act", lambda e: e.activation(junk[:], sg_f[:], AF.Square,
                                                           accum_out=ssg[:, t:t + 1]),
                             reads=[t_sgf], writes=[t_junk, t_ssg])

                def stageB1b(t, hg=hg):
                    own = t < 16
                    s = t % 2
                    do_g = own and hg == 0
                    if do_g:
                        P.op("act", lambda e: e.activation(
                            rg[:, t:t + 1], ssg[:, t:t + 1], AF.Sqrt, scale=1.0 / 512.0, bias=epsc[:, 1:2]),
                            reads=[t_ssg, t_eps], writes=[t_rg])
                        P.op("dve", lambda e: e.reciprocal(rg[:, t:t + 1], rg[:, t:t + 1]),
                             reads=[t_rg], writes=[t_rg])
                        P.op("dve", lambda e: e.scalar_tensor_tensor(
                            sgg[:, s, :], sg_f[:], rg[:, t:t + 1], gog[:], ALU.mult, ALU.mult),
                            reads=[t_sgf, t_rg, t_tab], writes=[t_sgg[s]])

                def stageB2(t, hg=hg):
                    own = t < 16
                    s = t % 2
                    do_g = own and hg == 0
                    if do_g:
                        for c in range(4):
                            P.op("pe", lambda e, c=c: e.transpose(
                                B7[:, 512 + c * 128:512 + (c + 1) * 128], sgg[:, s, c * 128:(c + 1) * 128],
                                ident[:]),
                                reads=[t_sgg[s], t_ident], writes=[tB7b])
                        P.op("act", lambda e: e.copy(
                            sgT[:, :, t * 128:(t + 1) * 128],
                            B7[:, 512:1024].rearrange("p (c k) -> p c k", k=128)),
                            reads=[tB7b], writes=[t_sgT])

                for t in range(2):
                    xload(t)
                stageX(0)
                stageX(1)
                stageA(0)
                stageA2(0)
                stageA2b(0)
                for t in range(NT):
                    if t + 2 < NT:
                        stageX(t + 2)
                    if t + 1 < NT:
                        stageA(t + 1)
                    stageB(t)
                    if t + 1 < NT:
                        stageA2(t + 1)
                    stageB1b(t)
                    if t >= 1:
                        stageB2(t - 1)
                    if t + 1 < NT:
                        stageA2b(t + 1)
                stageB2(NT - 1)

                if hg == 0:
                    BAR.extend(P.last_ops())
                kv = kms2[:].rearrange("p (n two) h -> p n two h", two=2)
                P.op("dve", lambda e: e.tensor_tensor(
                    kms[:].rearrange("p h n -> p n h"), kv[:, :, 0, :], kv[:, :, 1, :], ALU.add),
                    reads=[t_kms2], writes=[t_kms])
                P.op("dve", lambda e: e.tensor_scalar(km_bf[:], kms[:], 1.0 / 256.0, None, ALU.mult),
                     reads=[t_kms], writes=[t_km])

                rot = {"s": 0, "p": 0}
                deferred = []

                def gate_prep(Tq, hg=hg):
                    qs = Tq % 2
                    for half, bank, tb in ((0, B6, [tB6]), (1, B7, [tB7])):
                        for jj in range(2):
                            j = half * 2 + jj
                            tt = Tq * 4 + j
                            for h in range(4):
                                P.op("pe", lambda e, bank=bank, jj=jj, h=h, tt=tt: e.transpose(
                                    bank[0:64, (jj * 4 + h) * 128:(jj * 4 + h + 1) * 128],
                                    QB[:, tt, h, 0:64], ident[:]),
                                    reads=[t_QB, t_ident], writes=tb)
                        P.op("act", lambda e, bank=bank, half=half: e.copy(
                            QaT[0:64, qs, :, half * 256:(half + 1) * 256].rearrange("p h (j k) -> p j h k", k=128),
                            bank[0:64, :].rearrange("p (j h k) -> p j h k", h=4, k=128)),
                            reads=tb, writes=[t_QaT[qs]], extra=BAR)
                    for j in range(4):
                        for h in range(4):
                            P.op("pe", lambda e, j=j, h=h: e.matmul(
                                B[5][:, (j * 4 + h) * 16:(j * 4 + h + 1) * 16],
                                QaT[0:64, qs, h, j * 128:(j + 1) * 128], km_bf[:, h, :], start=True, stop=True),
                                reads=[t_QaT[qs], t_km], writes=[tB[5]])
                    el_b = eligb[:, Tq * 4:Tq * 4 + 4, :].unsqueeze(2).to_broadcast([128, 4, 4, 16])
                    el_1 = elig01[:, Tq * 4:Tq * 4 + 4, :].unsqueeze(2).to_broadcast([128, 4, 4, 16])
                    ow_1 = own01[:, Tq * 4:Tq * 4 + 4, :].unsqueeze(2).to_broadcast([128, 4, 4, 16])
                    gm4 = gm[:].rearrange("p (j h) n -> p j h n", h=4)
                    sel4 = sel[:].rearrange("p (j h) n -> p j h n", h=4)
                    P.op("dve", lambda e: e.tensor_tensor(
                        gm4, B[5][:, 0:256].rearrange("p (j h n) -> p j h n", h=4, n=16), el_b, ALU.add),
                        reads=[tB[5], t_el], writes=[t_gm])
                    for jh in range(16):
                        P.op("dve", lambda e, jh=jh: e.max(top8[:, jh, :], gm[:, jh, :]),
                             reads=[t_gm], writes=[t_top8])
                    P.op("dve", lambda e: e.tensor_tensor(
                        sel[:], gm[:], top8[:, :, 2:3].to_broadcast([128, 16, 16]), ALU.is_ge),
                        reads=[t_gm, t_top8], writes=[t_sel])
                    P.op("dve", lambda e: e.tensor_tensor(sel4, sel4, el_1, ALU.mult),
                         reads=[t_sel, t_el], writes=[t_sel])
                    P.op("dve", lambda e: e.tensor_tensor(sel4, sel4, ow_1, ALU.max),
                         reads=[t_sel, t_el], writes=[t_sel])
                    P.op("dve", lambda e: e.tensor_scalar(
                        QB[:, Tq * 4:Tq * 4 + 4, :, 64:80], sel4, -1.0, -NEG, ALU.add, ALU.mult),
                        reads=[t_sel], writes=[t_QB])

                def qaug(Tq):
                    qs = Tq % 2
                    for half, bank, tb in ((0, B6, [tB6]), (1, B7, [tB7])):
                        for jj in range(2):
                            j = half * 2 + jj
                            tt = Tq * 4 + j
                            for h in range(4):
                                P.op("pe", lambda e, bank=bank, jj=jj, h=h, tt=tt: e.transpose(
                                    bank[0:80, (jj * 4 + h) * 128:(jj * 4 + h + 1) * 128],
                                    QB[:, tt, h, :], ident[:]),
                                    reads=[t_QB, t_ident], writes=tb)
                        P.op("act", lambda e, bank=bank, half=half: e.copy(
                            QaT[:, qs, :, half * 256:(half + 1) * 256].rearrange("p h (j k) -> p j h k", k=128),
                            bank[0:80, :].rearrange("p (j h k) -> p j h k", h=4, k=128)),
                            reads=tb, writes=[t_QaT[qs]], extra=BAR)

                mid_deferred = []

                def flush_mid():
                    for fn in mid_deferred:
                        fn()
                    del mid_deferred[:]

                def flush_deferred():
                    flush_mid()
                    for fn in deferred:
                        fn()
                    del deferred[:]

                def attn_head(Tq, h, hg=hg):
                    qs = Tq % 2
                    klist = []
                    for kb in list(range(0, 2 * Tq + 2)) + list(range(8, 8 + 2 * Tq + 2)):
                        c0 = 256 if kb in (2 * Tq + 1, 8 + 2 * Tq + 1) else 0
                        diag = kb in (2 * Tq, 2 * Tq + 1)
                        for kt in range(2):
                            klist.append((kb, kt, c0, diag))
                    pr, ee = h // 2, h % 2
                    ob = 3 + (h % 2)
                    num0 = 0 if ee == 0 else 64
                    den0 = 64 if ee == 0 else 0
                    nk = len(klist)

                    def emit_qk(i):
                        kb, kt, c0, diag = klist[i]
                        sbk = rot["s"] % 3
                        rot["s"] += 1
                        ktile = kb * 2 + kt
                        P.op("pe", lambda e: e.matmul(
                            B[sbk][:, c0:512], KT[0:80, h, ktile * 128:(ktile + 1) * 128],
                            QaT[:, qs, h, c0:512], start=True, stop=(not diag)),
                            reads=[t_KT, t_oh, t_QaT[qs]], writes=[tB[sbk]])
                        if diag:
                            d0 = 0 if kb == 2 * Tq else 256
                            P.op("pe", lambda e: e.matmul(
                                B[sbk][:, d0:d0 + 256], ident[:], cmask[:, kt, :], start=False, stop=True),
                                reads=[t_ident, t_cm], writes=[tB[sbk]])
                        return sbk

                    def emit_rest(i, sbk):
                        kb, kt, c0, diag = klist[i]
                        pb = rot["p"] % 4
                        rot["p"] += 1
                        ktile = kb * 2 + kt
                        st_, sp_ = (i == 0), (i == nk - 1)
                        P.op("act", lambda e: e.activation(
                            Pbuf[:, pb, c0:512], B[sbk][:, c0:512], AF.Exp, scale=0.125),
                            reads=[tB[sbk]], writes=[t_P[pb]])
                        P.op("pe", lambda e: e.matmul(
                            B[ob][:, c0:512], V2[:, ktile, pr, ee * 64:ee * 64 + 128], Pbuf[:, pb, c0:512],
                            start=st_, stop=sp_),
                            reads=[t_V2, t_P[pb]], writes=[tB[ob]])

                    def run(pre, next_qk):
                        sbl = list(pre)
                        nxt = []
                        for i in range(nk):
                            if i + 2 < nk:
                                sbl.append(emit_qk(i + 2))
                            elif next_qk is not None:
                                nxt.append(next_qk(i + 2 - nk))
                            emit_rest(i, sbl[i])
                            if i == 7:
                                flush_mid()
                            if hg == 0 and i % 6 == 3:
                                emit_conv(1)
                        P.op("dve", lambda e: e.reciprocal(
                            rec[num0:num0 + 64, :], B[ob][den0:den0 + 64, :]),
                            reads=[tB[ob]], writes=[t_rec], extra=BAR)
                        P.op("dve", lambda e: e.tensor_tensor(
                            a_pair[num0:num0 + 64, pr, :], B[ob][num0:num0 + 64, :], rec[num0:num0 + 64, :],
                            ALU.mult),
                            reads=[tB[ob], t_rec], writes=[t_ap[pr]])
                        if ee == 1:
                            gp = hg * 2 + pr

                            def pair_act():
                                P.op("dve", lambda e: e.tensor_tensor(
                                    sq_bf[:, pr, :], a_pair[:, pr, :], a_pair[:, pr, :], ALU.mult),
                                    reads=[t_ap[pr]], writes=[t_sqb[pr]], extra=BAR)
                                P.op("dve", lambda e: e.tensor_scalar(
                                    agT[:, gp, Tq * 512:(Tq + 1) * 512], a_pair[:, pr, :], agg[:, gp:gp + 1], None,
                                    ALU.mult),
                                    reads=[t_ap[pr], t_agg], writes=[t_agT])
                            mid_deferred.append(pair_act)

                            def ssmm(pr=pr):
                                P.op("pe", lambda e: e.matmul(
                                    B[5][:], ones_bf[:], sq_bf[:, pr, :], start=(pr == 0), stop=(pr == 1)),
                                    reads=[t_ones, t_sqb[pr]], writes=[tB[5]])
                            deferred.append(ssmm)
                            if pr == 1:
                                def ssacc(Tq=Tq):
                                    if hg == 0:
                                        P.op("dve", lambda e: e.tensor_copy(ssb[:, Tq * 512:(Tq + 1) * 512], B[5][:]),
                                             reads=[tB[5]], writes=[t_ssb], extra=BAR)
                                    else:
                                        P.op("dve", lambda e: e.tensor_tensor(
                                            ssb[:, Tq * 512:(Tq + 1) * 512], ssb[:, Tq * 512:(Tq + 1) * 512], B[5][:],
                                            ALU.add),
                                            reads=[tB[5], t_ssb], writes=[t_ssb])
                                deferred.append(ssacc)
                        return nxt
                    return emit_qk, run

                gate_prep(0)
                qaug(0)
                for Tq in range(4):
                    heads = [attn_head(Tq, h) for h in range(4)]
                    pre = [heads[0][0](0), heads[0][0](1)]
                    for h in range(4):
                        if h == 3 and Tq + 1 < 4:
                            gate_prep(Tq + 1)
                        pre = heads[h][1](pre, heads[h + 1][0] if h + 1 < 4 else None)
                        if h == 0:
                            flush_deferred()
                    if Tq + 1 < 4:
                        qaug(Tq + 1)
                flush_deferred()
                if hg == 0:
                    emit_conv(100)


            P.op("act", lambda e: e.activation(ssb[:], ssb[:], AF.Ln, scale=1.0 / 512.0, bias=epsc[:, 1:2]),
                 reads=[t_ssb, t_eps], writes=[t_ssb])
            P.op("act", lambda e: e.activation(ra_bf, ssb[:], AF.Exp, scale=-0.5),
                 reads=[t_ssb], writes=[t_rab] + t_P)
            for gp in range(4):
                P.op("dve", lambda e, gp=gp: e.tensor_tensor(agT[:, gp, :], agT[:, gp, :], ra_bf, ALU.mult),
                     reads=[t_rab, t_agT], writes=[t_agT])
            P.emit()
        nc.all_engine_barrier()
        for tk in tB + [tB6, tB7, t_sgT, t_agT, t_ident, t_ones, t_eps]:
            tk.readers = []
            tk.writers = []

        with ExitStack() as es2:
            P = Prog(nc, "b")
            sb2 = lambda n, s, d: sb(n, s, d, es2)
            wo = sb2("wo", [128, 8, D], BF16)
            wd = sb2("wd", [128, NFC, D], BF16)
            wg = sb2("wg", [128, 3, 8, 128], BF16)
            wu = sb2("wu", [128, 3, 8, 128], BF16)
            hT = sb2("hT", [128, NFC, 512], BF16)
            x1T = sb2("x1T", [128, 8, 512], BF16)
            xin = sb2("xin", [128, 1, D], F32)
            y = sb2("y", [128, 4, D], F32)
            x1 = sb2("x1", [128, 4, D], F32)
            x1b = sb2("x1b", [128, 4, D], BF16)
            st1 = sb2("st1", [128, 4, 16], F32)
            st2 = sb2("st2", [128, 4, 16], F32)
            sl = sb2("sl", [128, 1, 512], F32)
            ln1g = sb2("ln1g", [128, D], F32)
            ln1b = sb2("ln1b", [128, D], F32)
            ln2g = sb2("ln2g", [128, D], F32)
            ln2b = sb2("ln2b", [128, D], F32)
            y2 = y
            T = Tok
            t_wo, t_wd, t_tab2 = T("wo"), T("wd"), T("tab2")
            t_tab2b = T("tab2b")
            t_wgu = [T("wgu%d" % i) for i in range(3)]
            t_hT, t_x1T = T("hT"), T("x1T")
            t_xin = [T("xin0"), T("xin1")]
            t_y = [T("y%d" % i) for i in range(4)]
            t_x1 = [T("x1_%d" % i) for i in range(4)]
            t_x1b = [T("x1b%d" % i) for i in range(4)]
            t_st1 = T("st1")
            t_st2 = [T("st2_%d" % i) for i in range(4)]
            t_sl = [T("sl0")] * 2
            t_y2 = t_y

            P.dma("sp", y[:, 0, :], xs[0:128, :], "xg0_0", writes=[t_y[0]])
            P.dma("sp", wo[:], wo_s.rearrange("kc p n -> p kc n"), "wo", writes=[t_wo])
            for j in range(1, 4):
                P.dma("sp", y[:, j, :], xs[j * 128:(j + 1) * 128, :], "xg0_%d" % j, writes=[t_y[j]])
            P.dma("sp", ln1g[:], ln1g_d, "c2a", writes=[t_tab2])
            P.dma("sp", ln1b[:], ln1b_d, "c2b", writes=[t_tab2])
            P.dma("sp", ln2g[:], ln2g_d, "c2c", writes=[t_tab2b])
            P.dma("sp", ln2b[:], ln2b_d, "c2d", writes=[t_tab2b])
            wd_v = wd_s.rearrange("fc p n -> p fc n")
            EPS1 = LN_EPS / (ALPHA * ALPHA)

            def ln_chain(views, vtoks, stt, t_stt, cols, g_t, b_t, post):
                for v, tk, c in zip(views, vtoks, cols):
                    P.op("dve", lambda e, v=v, c=c: e.bn_stats(stt[:, c, 0:6], v[:, 0:512]),
                         reads=[tk], writes=[t_stt])
                    P.op("dve", lambda e, v=v, c=c: e.bn_stats(stt[:, c, 6:12], v[:, 512:1024]),
                         reads=[tk], writes=[t_stt])
                    P.op("dve", lambda e, c=c: e.bn_aggr(stt[:, c, 12:14], stt[:, c, 0:12]),
                         reads=[t_stt], writes=[t_stt])
                return lambda: ln_part2(views, vtoks, stt, t_stt, cols, g_t, b_t, post)

            def ln_part2(views, vtoks, stt, t_stt, cols, g_t, b_t, post):
                c0, c1 = cols[0], cols[-1] + 1
                P.op("act", lambda e: e.activation(stt[:, c0:c1, 14], stt[:, c0:c1, 13], AF.Sqrt, bias=epsc[:, 2:3]),
                     reads=[t_stt, t_eps], writes=[t_stt])
                P.op("dve", lambda e: e.reciprocal(stt[:, c0:c1, 14], stt[:, c0:c1, 14]),
                     reads=[t_stt], writes=[t_stt])
                P.op("dve", lambda e: e.scalar_tensor_tensor(
                    stt[:, c0:c1, 15], stt[:, c0:c1, 12], -1.0, stt[:, c0:c1, 14], ALU.mult, ALU.mult),
                    reads=[t_stt], writes=[t_stt])
                for i, (v, tk, c) in enumerate(zip(views, vtoks, cols)):
                    P.op("act", lambda e, v=v, c=c: e.activation(
                        v, v, AF.Identity, scale=stt[:, c, 14:15], bias=stt[:, c, 15:16]),
                        reads=[t_stt, tk], writes=[tk])
                    P.op("dve", lambda e, v=v: e.tensor_tensor(v, v, g_t[:], ALU.mult),
                         reads=[tk, t_tab2], writes=[tk])
                    P.op("dve", lambda e, v=v: e.tensor_tensor(v, v, b_t[:], ALU.add),
                         reads=[tk, t_tab2], writes=[tk])
                    post(i)

            def W_mm(g, j):
                tt = g * 4 + j
                s = tt % 2
                if g == 0:
                    xres, t_xres = y[:, j, :], t_y[j]
                else:
                    P.dma("sp", xin[:, 0, :], xs[tt * 128:(tt + 1) * 128, :], "xin0", writes=[t_xin[0]])
                    xres, t_xres = xin[:, 0, :], t_xin[0]
                for hf in range(2):
                    bk = s * 2 + hf
                    for kc in range(8):
                        lhs = agT[:, kc, tt * 128:(tt + 1) * 128] if kc < 4 else sgT[:, kc - 4, tt * 128:(tt + 1) * 128]
                        P.op("pe", lambda e, lhs=lhs, bk=bk, kc=kc, hf=hf: e.matmul(
                            B[bk][:], lhs, wo[:, kc, hf * 512:(hf + 1) * 512], start=(kc == 0), stop=(kc == 7)),
                            reads=[t_agT, t_sgT, t_wo], writes=[tB[bk]])
                    P.op("dve", lambda e, bk=bk, hf=hf: e.scalar_tensor_tensor(
                        x1[:, j, hf * 512:(hf + 1) * 512], B[bk][:], 1.0 / ALPHA, xres[:, hf * 512:(hf + 1) * 512],
                        ALU.mult, ALU.add),
                        reads=[tB[bk], t_xres], writes=[t_x1[j]])

            def W_chain(g, js):
                def post(i):
                    j = js[i]
                    P.op("act", lambda e: e.copy(x1b[:, j, :], x1[:, j, :]),
                         reads=[t_x1[j]], writes=[t_x1b[j]])
                return ln_chain([x1[:, j, :] for j in js], [t_x1[j] for j in js], st1, t_st1, list(js),
                                ln1g, ln1b, post)

            def W_tr(g, j):
                bank, tbk = (B6, tB6) if j % 2 == 0 else (B7, tB7)
                for kc in range(8):
                    P.op("pe", lambda e, kc=kc: e.transpose(
                        bank[:, kc * 128:(kc + 1) * 128], x1b[:, j, kc * 128:(kc + 1) * 128], ident[:]),
                        reads=[t_x1b[j], t_ident], writes=[tbk])
                P.op("act", lambda e: e.copy(
                    x1T[:, :, j * 128:(j + 1) * 128], bank[:].rearrange("p (c k) -> p c k", k=128)),
                    reads=[tbk], writes=[t_x1T])

            def G_stage(g, steps=()):
                steps = list(steps)
                for fc in range(NFC):
                    ws_ = (g * NFC + fc) % 3
                    P.dma("sp", wg[:, ws_, :, :], wg_s[fc], "wgu%d" % ws_, writes=[t_wgu[ws_]])
                    P.dma("sp", wu[:, ws_, :, :], wu_s[fc], "wgu%d" % ws_, writes=[t_wgu[ws_]])
                    if g == 0 and 2 <= fc < 13:
                        i = fc - 2
                        P.dma("sp", wd[:, i * 2:(i + 1) * 2, :], wd_v[:, i * 2:(i + 1) * 2, :], "wd",
                              writes=[t_wd])
                    for kc in range(8):
                        P.op("pe", lambda e, kc=kc, ws_=ws_: e.matmul(
                            B[4][:], wg[:, ws_, kc, :], x1T[:, kc, :], start=(kc == 0), stop=(kc == 7)),
                            reads=[t_wgu[ws_], t_x1T], writes=[tB[4]])
                    for kc in range(8):
                        P.op("pe", lambda e, kc=kc, ws_=ws_: e.matmul(
                            B[5][:], wu[:, ws_, kc, :], x1T[:, kc, :], start=(kc == 0), stop=(kc == 7)),
                            reads=[t_wgu[ws_], t_x1T], writes=[tB[5]])
                    P.op("act", lambda e: e.activation(sl[:, 0, :], B[4][:], AF.Silu),
                         reads=[tB[4]], writes=[t_sl[0]])
                    P.op("dve", lambda e, fc=fc: e.tensor_tensor(hT[:, fc, :], sl[:, 0, :], B[5][:], ALU.mult),
                         reads=[tB[5], t_sl[0]], writes=[t_hT])
                    for _ in range(2):
                        if steps:
                            steps.pop(0)()
                while steps:
                    steps.pop(0)()

            def Dn(g, j):
                tt = g * 4 + j
                s = tt % 2
                for hf in range(2):
                    bk = s * 2 + hf
                    for fc in range(NFC):
                        P.op("pe", lambda e, bk=bk, fc=fc, hf=hf: e.matmul(
                            B[bk][:], hT[:, fc, j * 128:(j + 1) * 128], wd[:, fc, hf * 512:(hf + 1) * 512],
                            start=(fc == 0), stop=(fc == NFC - 1)),
                            reads=[t_hT, t_wd], writes=[tB[bk]])
                    P.op("dve", lambda e, bk=bk, hf=hf: e.scalar_tensor_tensor(
                        y[:, j, hf * 512:(hf + 1) * 512], B[bk][:], 1.0 / ALPHA, x1[:, j, hf * 512:(hf + 1) * 512],
                        ALU.mult, ALU.add),
                        reads=[tB[bk], t_x1[j]], writes=[t_y[j]])
                v, tk, stt, tst, c = y[:, j, :], t_y[j], st2, t_st2[j], j
                steps = []
                steps.append(lambda: P.op("dve", lambda e: e.bn_stats(stt[:, c, 0:6], v[:, 0:512]),
                                          reads=[tk], writes=[tst]))
                steps.append(lambda: (
                    P.op("dve", lambda e: e.bn_stats(stt[:, c, 6:12], v[:, 512:1024]), reads=[tk], writes=[tst]),
                    P.op("dve", lambda e: e.bn_aggr(stt[:, c, 12:14], stt[:, c, 0:12]), reads=[tst], writes=[tst])))
                steps.append(lambda: P.op(
                    "act", lambda e: e.activation(stt[:, c, 14:15], stt[:, c, 13:14], AF.Sqrt, bias=epsc[:, 2:3]),
                    reads=[tst, t_eps], writes=[tst]))
                steps.append(lambda: (
                    P.op("dve", lambda e: e.reciprocal(stt[:, c, 14:15], stt[:, c, 14:15]), reads=[tst], writes=[tst]),
                    P.op("dve", lambda e: e.scalar_tensor_tensor(
                        stt[:, c, 15:16], stt[:, c, 12:13], -1.0, stt[:, c, 14:15], ALU.mult, ALU.mult),
                        reads=[tst], writes=[tst])))
                steps.append(lambda: P.op(
                    "act", lambda e: e.activation(v, v, AF.Identity, scale=stt[:, c, 14:15], bias=stt[:, c, 15:16]),
                    reads=[tst, tk], writes=[tk]))
                steps.append(lambda: P.op("dve", lambda e: e.tensor_tensor(v, v, ln2g[:], ALU.mult),
                                          reads=[tk, t_tab2b], writes=[tk]))
                steps.append(lambda: (
                    P.op("dve", lambda e: e.tensor_tensor(v, v, ln2b[:], ALU.add), reads=[tk, t_tab2b], writes=[tk]),
                    P.dma("pool", out_d[tt * 128:(tt + 1) * 128, :], v, "out", reads=[tk])))
                return steps

            def round_robin(step_lists):
                out = []
                n = max(len(x) for x in step_lists)
                for i in range(n):
                    for x in step_lists:
                        if i < len(x):
                            out.append(x[i])
                return out

            for _ in range(24):
                P.op("pe", lambda e: e.matmul(B[5][:], ident[:], agT[:, 0, 0:512], start=True, stop=True),
                     reads=[t_ident, t_agT], writes=[tB[5]])
            W_mm(0, 0)
            c0 = W_chain(0, (0,))
            W_mm(0, 1)
            c0()
            c1 = W_chain(0, (1,))
            W_mm(0, 2)
            c1()
            c2 = W_chain(0, (2,))
            W_tr(0, 0)
            W_mm(0, 3)
            c2()
            c3 = W_chain(0, (3,))
            W_tr(0, 1)
            c3()
            W_tr(0, 2)
            W_tr(0, 3)
            pend = []
            for g in range(4):
                G_stage(g, pend)
                pend = []
                d0 = Dn(g, 0)
                d1 = Dn(g, 1)
                if g + 1 < 4:
                    W_mm(g + 1, 0)
                    W_mm(g + 1, 1)
                    cA = W_chain(g + 1, (0, 1))
                    d2 = Dn(g, 2)
                    cA()
                    W_mm(g + 1, 2)
                    c2 = W_chain(g + 1, (2,))
                    d3 = Dn(g, 3)
                    W_tr(g + 1, 0)
                    W_tr(g + 1, 1)
                    c2()
                    W_mm(g + 1, 3)
                    c3 = W_chain(g + 1, (3,))
                    W_tr(g + 1, 2)
                    c3()
                    W_tr(g + 1, 3)
                    pend = round_robin([d0, d1, d2, d3])
                else:
                    d2 = Dn(g, 2)
                    for st_ in round_robin([d0, d1]):
                        st_()
                    d3 = Dn(g, 3)
                    for st_ in round_robin([d2, d3]):
                        st_()
            P.emit()
    return nc


def _host_tables(e):
    own = A_BLK if e == 0 else B_BLK
    oth = B_BLK if e == 0 else A_BLK
    blocks = own + oth
    tok_idx = np.concatenate([np.arange(g * 256, (g + 1) * 256) for g in blocks])
    inv_freq = (10000.0 ** (-np.arange(0, 64, 2, dtype=np.float32) / 64.0)).astype(np.float32)
    ang = tok_idx.astype(np.float32)[:, None] * inv_freq[None, :]
    cos = np.cos(ang).astype(np.float32)
    sin = np.sin(ang).astype(np.float32)
    cs = np.concatenate([cos, -sin, sin], axis=1)
    csT = np.ascontiguousarray(cs.reshape(NT, 128, 96).transpose(1, 0, 2))
    eligb = np.zeros((16, 16), np.float32)
    elig01 = np.zeros((16, 16), np.float32)
    own01 = np.zeros((16, 16), np.float32)
    for tt in range(16):
        i = tt // 2
        gi = own[i]
        for kb in range(16):
            el = blocks[kb] < gi
            eligb[tt, kb] = 0.0 if el else -1e30
            elig01[tt, kb] = 1.0 if el else 0.0
            own01[tt, kb] = 1.0 if kb == i else 0.0
    bc = lambda a: np.ascontiguousarray(np.broadcast_to(a[None], (128,) + a.shape))
    return tok_idx, csT, bc(eligb), bc(elig01), bc(own01)


def _const_tables():
    onehot = np.zeros((16, S), np.float32)
    for kb in range(16):
        onehot[kb, kb * 256:(kb + 1) * 256] = 1.0
    cmask = np.zeros((128, 2, 256), np.float32)
    p = np.arange(128)[:, None]
    qq = np.arange(256)[None, :]
    for kt in range(2):
        cmask[:, kt, :] = np.where(kt * 128 + p <= qq, 0.0, NEG)
    ident = np.eye(128, dtype=np.float32)
    j = np.arange(128)[:, None]
    i = np.arange(128)[None, :]
    tril = (j <= i).astype(np.float32)
    bf = ml_dtypes.bfloat16
    return onehot.astype(bf), cmask.astype(bf), ident.astype(bf), tril.astype(bf)


_NC_CACHE = {}


def kernel(x, w_in, attn_out_g, gmlp_out_g, gmlp_ln_g, gmlp_ln_b, w_spatial, b_spatial,
           w_out, ln1_g, ln1_b, w_gate, w_up, w_down, ln2_g, ln2_b):
    f = lambda a: np.ascontiguousarray(np.asarray(a, dtype=np.float32))
    x = f(x)
    bc = lambda v, n: np.ascontiguousarray(np.broadcast_to(f(v).reshape(1, n), (128, n)))
    onehot, cmask, ident, tril = _const_tables()
    shared = {
        "w_in": f(w_in[0]), "w_out": f(w_out[0]), "w_gate": f(w_gate[0]), "w_up": f(w_up[0]),
        "w_down": f(w_down[0]),
        "onehot": onehot, "cmask": cmask, "ident": ident, "tril": tril,
        "wsT": np.ascontiguousarray(f(w_spatial[0]).transpose(2, 0, 1)),
        "bsT": np.ascontiguousarray(f(b_spatial[0]).T),
        "lng_b": bc(gmlp_ln_g[0], 512), "lnb_b": bc(gmlp_ln_b[0], 512), "gog_b": bc(gmlp_out_g[0], 512),
        "ag_fm": np.ascontiguousarray(f(attn_out_g[0]).reshape(4, 128).T),
        "ln1g_b": bc(ln1_g[0], D), "ln1b_b": bc(ln1_b[0], D),
        "ln2g_b": bc(ln2_g[0], D), "ln2b_b": bc(ln2_b[0], D),
    }
    in_maps = []
    idxs = []
    for c in range(8):
        b, e = c // 2, c % 2
        tok_idx, csT, eligb, elig01, own01 = _host_tables(e)
        idxs.append(tok_idx)
        m = dict(shared)
        m.update({"xs": np.ascontiguousarray(x[b][tok_idx]), "csT": csT,
                  "eligb": eligb, "elig01": elig01, "own01": own01})
        in_maps.append(m)
    if "nc" not in _NC_CACHE:
        _NC_CACHE["nc"] = build_program()
    nc = _NC_CACHE["nc"]
    res = run_bass_kernel_spmd(nc, in_maps, core_ids=list(range(8)))
    out = np.empty((4, S, D), np.float32)
    for c in range(8):
        b = c // 2
        out[b][idxs[c][:2048]] = res.results[c]["out"]
    return out
```

```python
import numpy as np
import ml_dtypes
from contextlib import ExitStack
import concourse.bass as bass
import concourse.mybir as mybir
from concourse.bass_utils import run_bass_kernel_spmd

F32 = mybir.dt.float32
BF16 = mybir.dt.bfloat16
AF = mybir.ActivationFunctionType
ALU = mybir.AluOpType
AX = mybir.AxisListType

D = 1024
S = 4096
NT = 32
FF = 2816
NFC = 22
ALPHA = 2.0 ** 0.25
LN_EPS = 1e-5
RMS_EPS = 1e-6
NEG = -30000.0
A_BLK = [0, 3, 4, 7, 8, 11, 12, 15]
B_BLK = [1, 2, 5, 6, 9, 10, 13, 14]


class Tok:
    __slots__ = ("name", "writers", "readers")

    def __init__(self, name=""):
        self.name = name
        self.writers = []
        self.readers = []


class Op:
    __slots__ = ("eng", "fn", "deps", "signal", "sig", "idx", "dma_sem", "dma_val")

    def __init__(self, eng, fn, idx):
        self.eng = eng
        self.fn = fn
        self.deps = []
        self.signal = False
        self.sig = None
        self.idx = idx
        self.dma_sem = None
        self.dma_val = None


class Prog:
    def __init__(self, nc, tag):
        self.nc = nc
        self.tag = tag
        self.ops = []
        self.stream = {"pe": [], "act": [], "dve": [], "pool": [], "sp": []}
        self.dma_list = {}

    def _add_deps(self, op, reads, writes, extra):
        deps = list(extra)
        for t in reads:
            deps.extend(t.writers)
        for t in writes:
            deps.extend(t.readers)
            deps.extend(t.writers)
        latest = {}
        out = []
        for d in deps:
            if d is op:
                continue
            if d.dma_sem is not None:
                if d not in out:
                    out.append(d)
                continue
            if d.eng == "pe" and op.eng == "pe" and op.dma_sem is None:
                continue
            cur = latest.get(d.eng)
            if cur is None or d.idx > cur.idx:
                latest[d.eng] = d
        out.extend(latest.values())
        for d in out:
            d.signal = True
        op.deps = out
        for t in reads:
            self._push(t.readers, op)
        for t in writes:
            if t.readers:
                t.readers = []
                t.writers = [op]
            else:
                self._push(t.writers, op)

    @staticmethod
    def _push(lst, op):
        if op.dma_sem is None:
            for i, o in enumerate(lst):
                if o.dma_sem is None and o.eng == op.eng:
                    lst[i] = op
                    return
        lst.append(op)

    def op(self, eng, fn, reads=(), writes=(), extra=()):
        o = Op(eng, fn, len(self.ops))
        self.ops.append(o)
        self.stream[eng].append(o)
        self._add_deps(o, reads, writes, extra)
        return o

    def dma(self, queue, out, in_, sem_name, reads=(), writes=(), extra=()):
        o = Op(queue, None, len(self.ops))
        o.dma_sem = sem_name
        lst = self.dma_list.setdefault(sem_name, [])
        lst.append(o)
        o.dma_val = 16 * len(lst)
        o.fn = lambda e: e.dma_start(out=out, in_=in_)
        self.ops.append(o)
        self.stream[queue].append(o)
        self._add_deps(o, reads, writes, extra)
        return o

    def last_ops(self):
        return [lst[-1] for k, lst in self.stream.items() if lst and k != "sp"]

    def emit(self):
        nc = self.nc
        for eng, lst in self.stream.items():
            c = 0
            for o in lst:
                if o.dma_sem is None and o.signal:
                    c += 1
                    o.sig = c
        with ExitStack() as es:
            sems = {e: es.enter_context(nc.semaphore(self.tag + "s_" + e)) for e in self.stream}
            dsems = {n: es.enter_context(nc.semaphore(self.tag + "d_" + n)) for n in self.dma_list}
            block = es.enter_context(nc.Block())
            streams = self.stream
            dma_list = self.dma_list

            def run(engname):
                def body(e):
                    waited = {}
                    for o in streams[engname]:
                        for d in o.deps:
                            if d.dma_sem is not None:
                                key = ("d", d.dma_sem)
                                val = 16 * sum(1 for x in dma_list[d.dma_sem] if x.idx < o.idx)
                                sem = dsems[d.dma_sem]
                            else:
                                key = ("c", d.eng)
                                val = d.sig
                                sem = sems[d.eng]
                            if waited.get(key, 0) >= val:
                                continue
                            waited[key] = val
                            e.wait_ge(sem, val)
                        ins = o.fn(e)
                        if o.dma_sem is not None:
                            ins.then_inc(dsems[o.dma_sem], 16)
                        elif o.signal:
                            ins.then_inc(sems[engname], 1)
                    mine = set(o.dma_sem for o in streams[engname] if o.dma_sem is not None)
                    for n in mine:
                        tot = 16 * len(dma_list[n])
                        if waited.get(("d", n), 0) < tot:
                            e.wait_ge(dsems[n], tot)
                return body

            block.tensor(run("pe"))
            block.scalar(run("act"))
            block.vector(run("dve"))
            block.gpsimd(run("pool"))
            block.sync(run("sp"))


def build_program(debug=False):
    nc = bass.Bass("TRN2", target_bir_lowering=False)

    def din(name, shape):
        return nc.dram_tensor(name, list(shape), F32, kind="ExternalInput").ap()

    xs = din("xs", [S, D])
    w_in = din("w_in", [D, 2560])
    w_out = din("w_out", [D, D])
    w_gate = din("w_gate", [D, FF])
    w_up = din("w_up", [D, FF])
    w_down = din("w_down", [FF, D])
    csT_d = din("csT", [128, NT, 96])
    eligb_d = din("eligb", [128, 16, 16])
    elig01_d = din("elig01", [128, 16, 16])
    own01_d = din("own01", [128, 16, 16])
    def din_bf(name, shape):
        return nc.dram_tensor(name, list(shape), BF16, kind="ExternalInput").ap()

    onehot_d = din_bf("onehot", [16, S])
    cmask_d = din_bf("cmask", [128, 2, 256])
    ident_d = din_bf("ident", [128, 128])
    tril_d = din_bf("tril", [128, 128])
    wsT_d = din("wsT", [128, 8, 128])
    bsT_d = din("bsT", [128, 8])
    lng_d = din("lng_b", [128, 512])
    lnb_d = din("lnb_b", [128, 512])
    gog_d = din("gog_b", [128, 512])
    agg_d = din("ag_fm", [128, 4])
    ln1g_d = din("ln1g_b", [128, D])
    ln1b_d = din("ln1b_b", [128, D])
    ln2g_d = din("ln2g_b", [128, D])
    ln2b_d = din("ln2b_b", [128, D])
    out_d = nc.dram_tensor("out", [2048, D], F32, kind="ExternalOutput").ap()

    w_in_v = w_in.rearrange("(kc p) n -> p kc n", p=128)
    wo_s = nc.dram_tensor("wo_s", [8, 128, D], BF16, kind="Internal").ap()
    wg_s = nc.dram_tensor("wg_s", [NFC, 128, 8, 128], BF16, kind="Internal").ap()
    wu_s = nc.dram_tensor("wu_s", [NFC, 128, 8, 128], BF16, kind="Internal").ap()
    wd_s = nc.dram_tensor("wd_s", [NFC, 128, D], BF16, kind="Internal").ap()

    with ExitStack() as es0:
        def sb(name, shape, dt, es=es0):
            return es.enter_context(nc.sbuf_tensor("s_" + name, list(shape), dt))

        def ps(name, shape, dt):
            return es0.enter_context(nc.psum_tensor("p_" + name, list(shape), dt))

        B = [ps("B%d" % i, [128, 512], F32) for i in range(6)]
        B6 = ps("B6", [128, 1024], BF16)
        B7 = ps("B7", [128, 1024], BF16)
        tB = [Tok("B%d" % i) for i in range(6)]
        tB6, tB7 = Tok("B6"), Tok("B7")
        tB7b = tB7

        sgT = sb("sgT", [128, 4, 2048], BF16)
        agT = sb("agT", [128, 4, 2048], BF16)
        ident = sb("ident", [128, 128], BF16)
        ones_bf = sb("ones_bf", [128, 128], BF16)
        epsc = sb("epsc", [128, 4], F32)
        t_sgT, t_agT, t_ident, t_ones = Tok("sgT"), Tok("agT"), Tok("ident"), Tok("ones")
        t_eps = Tok("eps")

        with ExitStack() as es1:
            P = Prog(nc, "a")
            sb1 = lambda n, s, d: sb(n, s, d, es1)
            KT = sb1("KT", [80, 4, S], BF16)
            V2 = sb1("V2", [128, NT, 2, 192], BF16)
            QB = sb1("QB", [128, 16, 4, 80], BF16)
            ov1 = sb1("ov1", [128, 8192], BF16)
            ov2 = sb1("ov2", [128, 4096], BF16)
            QaT = ov2[0:80, :].rearrange("p (a h k) -> p a h k", a=2, h=4)
            BAR = []
            Pbuf = sb1("Pbuf", [128, 4, 512], BF16)
            wqkv = sb1("wqkv", [128, 8, 768], BF16)
            wuz = ov1[:, :].rearrange("p (a b) -> p a b", a=8)
            xb = sb1("xb", [128, 4, D], BF16)
            xT = sb1("xT", [128, 2, 8, 128], BF16)
            cs = sb1("cs", [128, 2, 96], F32)
            eligb = sb1("eligb", [128, 16, 16], F32)
            elig01 = sb1("elig01", [128, 16, 16], F32)
            own01 = sb1("own01", [128, 16, 16], F32)
            cmask = sb1("cmask", [128, 2, 256], BF16)
            tril = sb1("tril", [128, 128], BF16)
            ws_bf = sb1("ws_bf", [128, 8, 128], BF16)
            bsT = sb1("bsT", [128, 8], F32)
            lng = sb1("lng", [128, 512], F32)
            lnb = sb1("lnb", [128, 512], F32)
            gog = sb1("gog", [128, 512], F32)
            agg = sb1("agg", [128, 4], F32)
            qk_f = sb1("qk_f", [128, 1, 512], F32)
            rt1 = sb1("rt1", [128, 512], F32)
            rt2 = sb1("rt2", [128, 512], F32)
            kr_bf = sb1("kr_bf", [128, 2, 256], BF16)
            gu = sb1("gu", [128, 2, 512], F32)
            gz = ov2[:, 0:1024].bitcast(F32)
            cen = ov2[:, 1024:2048].bitcast(F32)
            sq = ov2[:, 2048:3072].bitcast(F32)
            m8 = sb1("m8", [128, 8], F32)
            v8 = sb1("v8", [128, 8], F32)
            zn_bf = sb1("zn_bf", [128, 2, 512], BF16)
            sg_f = ov2[:, 3072:4096].bitcast(F32)
            junk = sb1("junk", [128, 512], BF16)
            ssg = sb1("ssg", [128, 16], F32)
            rg = sb1("rg", [128, 16], F32)
            sgg = sb1("sgg", [128, 2, 512], BF16)
            kms = sb1("kms", [64, 4, 16], F32)
            kms2 = sb1("kms2", [64, NT, 4], F32)
            km_bf = sb1("km_bf", [64, 4, 16], BF16)
            gm = sb1("gm", [128, 16, 16], F32)
            top8 = sb1("top8", [128, 16, 8], F32)
            sel = sb1("sel", [128, 16, 16], F32)
            ssb = ov1[:, 0:4096].bitcast(F32)
            a_pair = ov1[:, 4096:6144].bitcast(F32).rearrange("p (a b) -> p a b", a=2)
            rec = ov1[:, 6144:7168].bitcast(F32)
            sq_bf = ov1[:, 7168:8192].rearrange("p (a b) -> p a b", a=2)
            ra_bf = Pbuf[:, :, :].rearrange("p a b -> p (a b)")

            T = Tok
            t_KT, t_V2, t_QB, t_w, t_wuz, t_tab = T("KT"), T("V2"), T("QB"), T("wqkv"), T("wuz"), T("tab")
            t_oh = T("oh")
            t_cm, t_el, t_agg = T("cm"), T("el"), T("agg")
            t_QaT = [T("QaT0"), T("QaT1")]
            t_P = [T("P%d" % i) for i in range(4)]
            t_xb = [T("xb%d" % i) for i in range(4)]
            t_xT = [T("xT0"), T("xT1")]
            t_qkf = [T("qkf0")]
            t_cs = [T("cs0"), T("cs1")]
            t_rt1, t_rt2 = T("rt1"), T("rt2")
            t_kr = [T("kr0"), T("kr1")]
            t_gu = [T("gu0"), T("gu1")]
            t_gz, t_cen, t_sq, t_m8, t_v8 = T("gz"), T("cen"), T("sq"), T("m8"), T("v8")
            t_zn = [T("zn0"), T("zn1")]
            t_sgf, t_junk, t_ssg, t_rg = T("sgf"), T("junk"), T("ssg"), T("rg")
            t_sgg = [T("sgg0"), T("sgg1")]
            t_ws, t_wsf = T("ws"), T("wsf")
            t_kms, t_km = T("kms"), T("km")
            t_kms2 = T("kms2")
            t_gm, t_top8, t_sel = T("gm"), T("top8"), T("sel")
            t_rec = T("rec")
            t_ap = [T("ap0"), T("ap1")]
            t_sqb = [T("sqb0"), T("sqb1")]
            t_ssb, t_rab = T("ssb"), T("rab")

            kc_ = [0]

            def cdma(queue, out, in_, **kw):
                kc_[0] += 1
                return P.dma(queue, out, in_, "k%d" % kc_[0], **kw)

            cdma("sp", ident[:], ident_d, writes=[t_ident])
            cdma("sp", cmask[:], cmask_d, writes=[t_cm])
            cdma("sp", eligb[:], eligb_d, writes=[t_el])
            cdma("sp", elig01[:], elig01_d, writes=[t_el])
            cdma("sp", own01[:], own01_d, writes=[t_el])
            cdma("sp", tril[:], tril_d, writes=[t_wsf])
            cdma("sp", bsT[:], bsT_d, writes=[t_tab])
            cdma("sp", lng[:], lng_d, writes=[t_tab])
            cdma("sp", lnb[:], lnb_d, writes=[t_tab])
            cdma("sp", gog[:], gog_d, writes=[t_tab])
            cdma("sp", agg[:], agg_d, writes=[t_agg])
            for h in range(4):
                cdma("sp", KT[64:80, h, :], onehot_d, writes=[t_oh])
            P.op("pool", lambda e: e.memset(ones_bf[:], 1.0), writes=[t_ones])
            P.op("pool", lambda e: e.memset(epsc[:, 0:1], LN_EPS), writes=[t_eps])
            P.op("pool", lambda e: e.memset(epsc[:, 1:2], RMS_EPS), writes=[t_eps])
            P.op("pool", lambda e: e.memset(epsc[:, 2:3], LN_EPS / (ALPHA * ALPHA)), writes=[t_eps])
            P.op("pool", lambda e: e.memset(V2[:, :, :, 64:128], 1.0), writes=[t_V2])
            P.op("dve", lambda e: e.memset(ssg[:], 0.0), writes=[t_ssg])

            conv = []
            conv.append((wo_s, w_out.rearrange("(kc p) n -> kc p n", p=128)))
            wg_src = w_gate.rearrange("(kc p) (fc f) -> fc p kc f", p=128, f=128)
            wu_src = w_up.rearrange("(kc p) (fc f) -> fc p kc f", p=128, f=128)
            for fc in range(NFC):
                conv.append((wg_s[fc], wg_src[fc]))
                conv.append((wu_s[fc], wu_src[fc]))
            wd_src = w_down.rearrange("(fc p) n -> fc p n", p=128)
            conv.append((wd_s[0:11], wd_src[0:11]))
            conv.append((wd_s[11:22], wd_src[11:22]))
            conv_it = iter(conv)

            def emit_conv(n):
                for _ in range(n):
                    c = next(conv_it, None)
                    if c is not None:
                        P.dma("pool", c[0], c[1], "cv")

            for hg in range(2):
                for j, base in enumerate((0, 512, 1024)):
                    c0 = base + hg * 256
                    P.dma("pool", wqkv[:, :, j * 256:(j + 1) * 256], w_in_v[:, :, c0:c0 + 256], "w_in",
                          writes=[t_w])
                if hg == 0:
                    P.dma("pool", wuz[:], w_in_v[:, :, 1536:2560], "w_in", writes=[t_wuz])
                    cdma("pool", ws_bf[:], wsT_d, writes=[t_wsf])
                    P.op("dve", lambda e: e.tensor_tensor(
                        ws_bf[:], ws_bf[:], tril[:].unsqueeze(1).to_broadcast([128, 8, 128]), ALU.mult),
                        reads=[t_wsf], writes=[t_ws, t_wsf])

                def xload(t):
                    P.dma("pool", xb[:, t % 4, :], xs[t * 128:(t + 1) * 128, :], "x%d" % (t % 4),
                          writes=[t_xb[t % 4]])

                def stageX(t, hg=hg):
                    s = t % 2
                    s4 = t % 4
                    if t + 2 < NT:
                        xload(t + 2)
                    for kc in range(8):
                        P.op("pe", lambda e, kc=kc: e.transpose(
                            B6[:, kc * 128:(kc + 1) * 128], xb[:, s4, kc * 128:(kc + 1) * 128], ident[:]),
                            reads=[t_xb[s4], t_ident], writes=[tB6])
                    P.op("act", lambda e: e.copy(xT[:, s, :, :].rearrange("p a b -> p (a b)"), B6[:]),
                         reads=[tB6], writes=[t_xT[s]])

                def stageA(t, hg=hg):
                    own = t < 16
                    s = t % 2
                    do_g = own and hg == 0
                    rope_eng = "pool" if (hg == 0 and own) else "dve"
                    P.dma("sp", cs[:, s, :], csT_d[:, t, :], "cs%d" % s, writes=[t_cs[s]])
                    c_lo = 0 if own else 256
                    for kc in range(8):
                        P.op("pe", lambda e, kc=kc: e.matmul(
                            B[0][:, c_lo:512], xT[:, s, kc, :], wqkv[:, kc, c_lo:512],
                            start=(kc == 0), stop=(kc == 7)),
                            reads=[t_xT[s], t_w], writes=[tB[0]])
                    for kc in range(8):
                        P.op("pe", lambda e, kc=kc: e.matmul(
                            B[1][:, 0:256], xT[:, s, kc, :], wqkv[:, kc, 512:768],
                            start=(kc == 0), stop=(kc == 7)),
                            reads=[t_xT[s], t_w], writes=[tB[1]])
                    if do_g:
                        for kc in range(8):
                            P.op("pe", lambda e, kc=kc: e.matmul(
                                B[2][:], xT[:, s, kc, :], wuz[:, kc, 0:512],
                                start=(kc == 0), stop=(kc == 7)),
                                reads=[t_xT[s], t_wuz], writes=[tB[2]])
                        for kc in range(8):
                            P.op("pe", lambda e, kc=kc: e.matmul(
                                B[3][:], xT[:, s, kc, :], wuz[:, kc, 512:1024],
                                start=(kc == 0), stop=(kc == 7)),
                                reads=[t_xT[s], t_wuz], writes=[tB[3]])
                    P.op("act", lambda e: e.copy(qk_f[:, 0, c_lo:512], B[0][:, c_lo:512]),
                         reads=[tB[0]], writes=[t_qkf[0]])
                    P.op("act", lambda e: e.copy(
                        V2[:, t, :, :].rearrange("p a (e c) -> p a e c", c=64)[:, :, 0:3:2, :],
                        B[1][:, 0:256].rearrange("p (a e c) -> p a e c", e=2, c=64)),
                        reads=[tB[1]], writes=[t_V2])
                    if do_g:
                        P.op("act", lambda e: e.activation(gu[:, s, :], B[2][:], AF.Gelu),
                             reads=[tB[2]], writes=[t_gu[s]])
                        P.op("act", lambda e: e.activation(gz[:], B[3][:], AF.Gelu),
                             reads=[tB[3]], writes=[t_gz])
                    nh = 8 if own else 4
                    qv = qk_f[:, 0, c_lo:512].rearrange("p (h d) -> p h d", d=64)
                    r2 = rt2[:, c_lo:512].rearrange("p (h d) -> p h d", d=64)
                    qv4 = qk_f[:, 0, c_lo:512].rearrange("p (h a d) -> p h a d", a=2, d=32)
                    r14 = rt1[:, c_lo:512].rearrange("p (h a d) -> p h a d", a=2, d=32)
                    cb = cs[:, s, 0:32].unsqueeze(1).unsqueeze(1).to_broadcast([128, nh, 2, 32])
                    sb_lo = cs[:, s, 32:64].unsqueeze(1).to_broadcast([128, nh, 32])
                    sb_hi = cs[:, s, 64:96].unsqueeze(1).to_broadcast([128, nh, 32])
                    P.op(rope_eng, lambda e: e.tensor_tensor(r14, qv4, cb, ALU.mult),
                         reads=[t_qkf[0], t_cs[s]], writes=[t_rt1])
                    P.op(rope_eng, lambda e: e.tensor_tensor(r2[:, :, 0:32], qv[:, :, 32:64], sb_lo, ALU.mult),
                         reads=[t_qkf[0], t_cs[s]], writes=[t_rt2])
                    P.op(rope_eng, lambda e: e.tensor_tensor(r2[:, :, 32:64], qv[:, :, 0:32], sb_hi, ALU.mult),
                         reads=[t_qkf[0], t_cs[s]], writes=[t_rt2])
                    if own:
                        P.op(rope_eng, lambda e: e.tensor_tensor(
                            QB[:, t, :, 0:64], rt1[:, 0:256].rearrange("p (h d) -> p h d", d=64),
                            rt2[:, 0:256].rearrange("p (h d) -> p h d", d=64), ALU.add),
                            reads=[t_rt1, t_rt2], writes=[t_QB])
                    P.op(rope_eng, lambda e: e.tensor_tensor(
                        kr_bf[:, s, :], rt1[:, 256:512], rt2[:, 256:512], ALU.add),
                        reads=[t_rt1, t_rt2], writes=[t_kr[s]])
                def stageA2(t, hg=hg):
                    own = t < 16
                    s = t % 2
                    do_g = own and hg == 0
                    if do_g:
                        gz3 = gz[:].rearrange("p (g d) -> p g d", d=64)
                        cen3 = cen[:].rearrange("p (g d) -> p g d", d=64)
                        sq3 = sq[:].rearrange("p (g d) -> p g d", d=64)
                        P.op("dve", lambda e: e.tensor_reduce(m8[:], gz3, AX.X, ALU.add),
                             reads=[t_gz], writes=[t_m8])
                        P.op("dve", lambda e: e.scalar_tensor_tensor(
                            cen3, m8[:].unsqueeze(2).to_broadcast([128, 8, 64]), -1.0 / 64.0, gz3,
                            ALU.mult, ALU.add),
                            reads=[t_gz, t_m8], writes=[t_cen])
                        P.op("dve", lambda e: e.tensor_tensor(sq[:], cen[:], cen[:], ALU.mult),
                             reads=[t_cen], writes=[t_sq])
                        P.op("dve", lambda e: e.tensor_reduce(v8[:], sq3, AX.X, ALU.add),
                             reads=[t_sq], writes=[t_v8])

                def stageA2b(t, hg=hg):
                    own = t < 16
                    s = t % 2
                    do_g = own and hg == 0
                    if do_g:
                        cen3 = cen[:].rearrange("p (g d) -> p g d", d=64)
                        sq3 = sq[:].rearrange("p (g d) -> p g d", d=64)
                        P.op("act", lambda e: e.activation(v8[:], v8[:], AF.Sqrt, scale=1.0 / 64.0, bias=epsc[:, 0:1]),
                             reads=[t_v8, t_eps], writes=[t_v8])
                        P.op("dve", lambda e: e.reciprocal(v8[:], v8[:]),
                             reads=[t_v8], writes=[t_v8])
                        P.op("dve", lambda e: e.tensor_tensor(
                            sq3, cen3, v8[:].unsqueeze(2).to_broadcast([128, 8, 64]), ALU.mult),
                            reads=[t_cen, t_v8], writes=[t_sq])
                        P.op("dve", lambda e: e.tensor_tensor(cen[:], sq[:], lng[:], ALU.mult),
                             reads=[t_sq, t_tab], writes=[t_cen])
                        P.op("dve", lambda e: e.tensor_tensor(zn_bf[:, s, :], cen[:], lnb[:], ALU.add),
                             reads=[t_cen, t_tab], writes=[t_zn[s]])

                def stageB(t, hg=hg):
                    own = t < 16
                    s = t % 2
                    do_g = own and hg == 0
                    for h in range(4):
                        P.op("pe", lambda e, h=h: e.transpose(
                            B7[0:64, h * 128:(h + 1) * 128], kr_bf[:, s, h * 64:(h + 1) * 64], ident[:]),
                            reads=[t_kr[s], t_ident], writes=[tB7])
                    P.op("dve", lambda e: e.tensor_copy(
                        KT[0:64, :, t * 128:(t + 1) * 128],
                        B7[0:64, 0:512].rearrange("p (h k) -> p h k", k=128)),
                        reads=[tB7], writes=[t_KT])
                    P.op("dve", lambda e: e.tensor_reduce(
                        kms2[:, t, :], B7[0:64, 0:512].rearrange("p (h k) -> p h k", k=128), AX.X, ALU.add),
                        reads=[tB7], writes=[t_kms2])
                    if do_g:
                        for g in range(8):
                            P.op("pe", lambda e, g=g: e.matmul(
                                B[4][:, g * 64:(g + 1) * 64], ws_bf[:, g, :], zn_bf[:, s, g * 64:(g + 1) * 64],
                                start=True, stop=True),
                                reads=[t_zn[s], t_ws], writes=[tB[4]])
                        P.op("dve", lambda e: e.tensor_tensor(
                            sg_f[:].rearrange("p (g d) -> p g d", d=64),
                            B[4][:].rearrange("p (g d) -> p g d", d=64),
                            bsT[:].unsqueeze(2).to_broadcast([128, 8, 64]), ALU.add),
                            reads=[tB[4], t_tab], writes=[t_sgf])
                        P.op("dve", lambda e: e.tensor_tensor(sg_f[:], sg_f[:], gu[:, s, :], ALU.mult),
                             reads=[t_sgf, t_gu[s]], writes=[t_sgf])
                        P.op("act", lambda e: e.activation(junk[:], sg_f[:], AF.Square,
                                                           accum_out=ssg[:, t:t + 1]),
                             reads=[t_sgf], writes=[t_junk, t_ssg])

                def stageB1b(t, hg=hg):
                    own = t < 16
                    s = t % 2
                    do_g = own and hg == 0
                    if do_g:
                        P.op("act", lambda e: e.activation(
                            rg[:, t:t + 1], ssg[:, t:t + 1], AF.Sqrt, scale=1.0 / 512.0, bias=epsc[:, 1:2]),
                            reads=[t_ssg, t_eps], writes=[t_rg])
                        P.op("dve", lambda e: e.reciprocal(rg[:, t:t + 1], rg[:, t:t + 1]),
                             reads=[t_rg], writes=[t_rg])
                        P.op("dve", lambda e: e.scalar_tensor_tensor(
                            sgg[:, s, :], sg_f[:], rg[:, t:t + 1], gog[:], ALU.mult, ALU.mult),
                            reads=[t_sgf, t_rg, t_tab], writes=[t_sgg[s]])

                def stageB2(t, hg=hg):
                    own = t < 16
                    s = t % 2
                    do_g = own and hg == 0
                    if do_g:
                        for c in range(4):
                            P.op("pe", lambda e, c=c: e.transpose(
                                B7[:, 512 + c * 128:512 + (c + 1) * 128], sgg[:, s, c * 128:(c + 1) * 128],
                                ident[:]),
                                reads=[t_sgg[s], t_ident], writes=[tB7b])
                        P.op("act", lambda e: e.copy(
                            sgT[:, :, t * 128:(t + 1) * 128],
                            B7[:, 512:1024].rearrange("p (c k) -> p c k", k=128)),
                            reads=[tB7b], writes=[t_sgT])

                for t in range(2):
                    xload(t)
                stageX(0)
                stageX(1)
                stageA(0)
                stageA2(0)
                stageA2b(0)
                for t in range(NT):
                    if t + 2 < NT:
                        stageX(t + 2)
                    if t + 1 < NT:
                        stageA(t + 1)
                    stageB(t)
                    if t + 1 < NT:
                        stageA2(t + 1)
                    stageB1b(t)
                    if t >= 1:
                        stageB2(t - 1)
                    if t + 1 < NT:
                        stageA2b(t + 1)
                stageB2(NT - 1)

                if hg == 0:
                    BAR.extend(P.last_ops())
                kv = kms2[:].rearrange("p (n two) h -> p n two h", two=2)
                P.op("dve", lambda e: e.tensor_tensor(
                    kms[:].rearrange("p h n -> p n h"), kv[:, :, 0, :], kv[:, :, 1, :], ALU.add),
                    reads=[t_kms2], writes=[t_kms])
                P.op("dve", lambda e: e.tensor_scalar(km_bf[:], kms[:], 1.0 / 256.0, None, ALU.mult),
                     reads=[t_kms], writes=[t_km])

                rot = {"s": 0, "p": 0}
                deferred = []

                def gate_prep(Tq, hg=hg):
                    qs = Tq % 2
                    for half, bank, tb in ((0, B6, [tB6]), (1, B7, [tB7])):
                        for jj in range(2):
                            j = half * 2 + jj
                            tt = Tq * 4 + j
                            for h in range(4):
                                P.op("pe", lambda e, bank=bank, jj=jj, h=h, tt=tt: e.transpose(
                                    bank[0:64, (jj * 4 + h) * 128:(jj * 4 + h + 1) * 128],
                                    QB[:, tt, h, 0:64], ident[:]),
                                    reads=[t_QB, t_ident], writes=tb)
                        P.op("act", lambda e, bank=bank, half=half: e.copy(
                            QaT[0:64, qs, :, half * 256:(half + 1) * 256].rearrange("p h (j k) -> p j h k", k=128),
                            bank[0:64, :].rearrange("p (j h k) -> p j h k", h=4, k=128)),
                            reads=tb, writes=[t_QaT[qs]], extra=BAR)
                    for j in range(4):
                        for h in range(4):
                            P.op("pe", lambda e, j=j, h=h: e.matmul(
                                B[5][:, (j * 4 + h) * 16:(j * 4 + h + 1) * 16],
                                QaT[0:64, qs, h, j * 128:(j + 1) * 128], km_bf[:, h, :], start=True, stop=True),
                                reads=[t_QaT[qs], t_km], writes=[tB[5]])
                    el_b = eligb[:, Tq * 4:Tq * 4 + 4, :].unsqueeze(2).to_broadcast([128, 4, 4, 16])
                    el_1 = elig01[:, Tq * 4:Tq * 4 + 4, :].unsqueeze(2).to_broadcast([128, 4, 4, 16])
                    ow_1 = own01[:, Tq * 4:Tq * 4 + 4, :].unsqueeze(2).to_broadcast([128, 4, 4, 16])
                    gm4 = gm[:].rearrange("p (j h) n -> p j h n", h=4)
                    sel4 = sel[:].rearrange("p (j h) n -> p j h n", h=4)
                    P.op("dve", lambda e: e.tensor_tensor(
                        gm4, B[5][:, 0:256].rearrange("p (j h n) -> p j h n", h=4, n=16), el_b, ALU.add),
                        reads=[tB[5], t_el], writes=[t_gm])
                    for jh in range(16):
                        P.op("dve", lambda e, jh=jh: e.max(top8[:, jh, :], gm[:, jh, :]),
                             reads=[t_gm], writes=[t_top8])
                    P.op("dve", lambda e: e.tensor_tensor(
                        sel[:], gm[:], top8[:, :, 2:3].to_broadcast([128, 16, 16]), ALU.is_ge),
                        reads=[t_gm, t_top8], writes=[t_sel])
                    P.op("dve", lambda e: e.tensor_tensor(sel4, sel4, el_1, ALU.mult),
                         reads=[t_sel, t_el], writes=[t_sel])
                    P.op("dve", lambda e: e.tensor_tensor(sel4, sel4, ow_1, ALU.max),
                         reads=[t_sel, t_el], writes=[t_sel])
                    P.op("dve", lambda e: e.tensor_scalar(
                        QB[:, Tq * 4:Tq * 4 + 4, :, 64:80], sel4, -1.0, -NEG, ALU.add, ALU.mult),
                        reads=[t_sel], writes=[t_QB])

                def qaug(Tq):
                    qs = Tq % 2
                    for half, bank, tb in ((0, B6, [tB6]), (1, B7, [tB7])):
                        for jj in range(2):
                            j = half * 2 + jj
                            tt = Tq * 4 + j
                            for h in range(4):
                                P.op("pe", lambda e, bank=bank, jj=jj, h=h, tt=tt: e.transpose(
                                    bank[0:80, (jj * 4 + h) * 128:(jj * 4 + h + 1) * 128],
                                    QB[:, tt, h, :], ident[:]),
                                    reads=[t_QB, t_ident], writes=tb)
                        P.op("act", lambda e, bank=bank, half=half: e.copy(
                            QaT[:, qs, :, half * 256:(half + 1) * 256].rearrange("p h (j k) -> p j h k", k=128),
                            bank[0:80, :].rearrange("p (j h k) -> p j h k", h=4, k=128)),
                            reads=tb, writes=[t_QaT[qs]], extra=BAR)

                mid_deferred = []

                def flush_mid():
                    for fn in mid_deferred:
                        fn()
                    del mid_deferred[:]

                def flush_deferred():
                    flush_mid()
                    for fn in deferred:
                        fn()
                    del deferred[:]

                def attn_head(Tq, h, hg=hg):
                    qs = Tq % 2
                    klist = []
                    for kb in list(range(0, 2 * Tq + 2)) + list(range(8, 8 + 2 * Tq + 2)):
                        c0 = 256 if kb in (2 * Tq + 1, 8 + 2 * Tq + 1) else 0
                        diag = kb in (2 * Tq, 2 * Tq + 1)
                        for kt in range(2):
                            klist.append((kb, kt, c0, diag))
                    pr, ee = h // 2, h % 2
                    ob = 3 + (h % 2)
                    num0 = 0 if ee == 0 else 64
                    den0 = 64 if ee == 0 else 0
                    nk = len(klist)

                    def emit_qk(i):
                        kb, kt, c0, diag = klist[i]
                        sbk = rot["s"] % 3
                        rot["s"] += 1
                        ktile = kb * 2 + kt
                        P.op("pe", lambda e: e.matmul(
                            B[sbk][:, c0:512], KT[0:80, h, ktile * 128:(ktile + 1) * 128],
                            QaT[:, qs, h, c0:512], start=True, stop=(not diag)),
                            reads=[t_KT, t_oh, t_QaT[qs]], writes=[tB[sbk]])
                        if diag:
                            d0 = 0 if kb == 2 * Tq else 256
                            P.op("pe", lambda e: e.matmul(
                                B[sbk][:, d0:d0 + 256], ident[:], cmask[:, kt, :], start=False, stop=True),
                                reads=[t_ident, t_cm], writes=[tB[sbk]])
                        return sbk

                    def emit_rest(i, sbk):
                        kb, kt, c0, diag = klist[i]
                        pb = rot["p"] % 4
                        rot["p"] += 1
                        ktile = kb * 2 + kt
                        st_, sp_ = (i == 0), (i == nk - 1)
                        P.op("act", lambda e: e.activation(
                            Pbuf[:, pb, c0:512], B[sbk][:, c0:512], AF.Exp, scale=0.125),
                            reads=[tB[sbk]], writes=[t_P[pb]])
                        P.op("pe", lambda e: e.matmul(
                            B[ob][:, c0:512], V2[:, ktile, pr, ee * 64:ee * 64 + 128], Pbuf[:, pb, c0:512],
                            start=st_, stop=sp_),
                            reads=[t_V2, t_P[pb]], writes=[tB[ob]])

                    def run(pre, next_qk):
                        sbl = list(pre)
                        nxt = []
                        for i in range(nk):
                            if i + 2 < nk:
                                sbl.append(emit_qk(i + 2))
                            elif next_qk is not None:
                                nxt.append(next_qk(i + 2 - nk))
                            emit_rest(i, sbl[i])
                            if i == 7:
                                flush_mid()
                            if hg == 0 and i % 6 == 3:
                                emit_conv(1)
                        P.op("dve", lambda e: e.reciprocal(
                            rec[num0:num0 + 64, :], B[ob][den0:den0 + 64, :]),
                            reads=[tB[ob]], writes=[t_rec], extra=BAR)
                        P.op("dve", lambda e: e.tensor_tensor(
                            a_pair[num0:num0 + 64, pr, :], B[ob][num0:num0 + 64, :], rec[num0:num0 + 64, :],
                            ALU.mult),
                            reads=[tB[ob], t_rec], writes=[t_ap[pr]])
                        if ee == 1:
                            gp = hg * 2 + pr

                            def pair_act():
                                P.op("act", lambda e: e.activation(sq_bf[:, pr, :], a_pair[:, pr, :], AF.Square),
                                     reads=[t_ap[pr]], writes=[t_sqb[pr]], extra=BAR)
                                P.op("dve", lambda e: e.tensor_scalar(
                                    agT[:, gp, Tq * 512:(Tq + 1) * 512], a_pair[:, pr, :], agg[:, gp:gp + 1], None,
                                    ALU.mult),
                                    reads=[t_ap[pr], t_agg], writes=[t_agT])
                            mid_deferred.append(pair_act)

                            def ssmm(pr=pr):
                                P.op("pe", lambda e: e.matmul(
                                    B[5][:], ones_bf[:], sq_bf[:, pr, :], start=(pr == 0), stop=(pr == 1)),
                                    reads=[t_ones, t_sqb[pr]], writes=[tB[5]])
                            deferred.append(ssmm)
                            if pr == 1:
                                def ssacc(Tq=Tq):
                                    if hg == 0:
                                        P.op("dve", lambda e: e.tensor_copy(ssb[:, Tq * 512:(Tq + 1) * 512], B[5][:]),
                                             reads=[tB[5]], writes=[t_ssb], extra=BAR)
                                    else:
                                        P.op("dve", lambda e: e.tensor_tensor(
                                            ssb[:, Tq * 512:(Tq + 1) * 512], ssb[:, Tq * 512:(Tq + 1) * 512], B[5][:],
                                            ALU.add),
                                            reads=[tB[5], t_ssb], writes=[t_ssb])
                                deferred.append(ssacc)
                        return nxt
                    return emit_qk, run

                gate_prep(0)
                qaug(0)
                for Tq in range(4):
                    heads = [attn_head(Tq, h) for h in range(4)]
                    pre = [heads[0][0](0), heads[0][0](1)]
                    for h in range(4):
                        if h == 3 and Tq + 1 < 4:
                            gate_prep(Tq + 1)
                        pre = heads[h][1](pre, heads[h + 1][0] if h + 1 < 4 else None)
                        if h == 0:
                            flush_deferred()
                    if Tq + 1 < 4:
                        qaug(Tq + 1)
                flush_deferred()
                if hg == 0:
                    emit_conv(100)


            P.op("act", lambda e: e.activation(ssb[:], ssb[:], AF.Ln, scale=1.0 / 512.0, bias=epsc[:, 1:2]),
                 reads=[t_ssb, t_eps], writes=[t_ssb])
            P.op("act", lambda e: e.activation(ra_bf, ssb[:], AF.Exp, scale=-0.5),
                 reads=[t_ssb], writes=[t_rab] + t_P)
            for gp in range(4):
                P.op("dve", lambda e, gp=gp: e.tensor_tensor(agT[:, gp, :], agT[:, gp, :], ra_bf, ALU.mult),
                     reads=[t_rab, t_agT], writes=[t_agT])
            P.emit()
        nc.all_engine_barrier()
        for tk in tB + [tB6, tB7, t_sgT, t_agT, t_ident, t_ones, t_eps]:
            tk.readers = []
            tk.writers = []

        with ExitStack() as es2:
            P = Prog(nc, "b")
            sb2 = lambda n, s, d: sb(n, s, d, es2)
            wo = sb2("wo", [128, 8, D], BF16)
            wd = sb2("wd", [128, NFC, D], BF16)
            wg = sb2("wg", [128, 3, 8, 128], BF16)
            wu = sb2("wu", [128, 3, 8, 128], BF16)
            hT = sb2("hT", [128, NFC, 512], BF16)
            x1T = sb2("x1T", [128, 8, 512], BF16)
            xin = sb2("xin", [128, 1, D], F32)
            y = sb2("y", [128, 4, D], F32)
            x1 = sb2("x1", [128, 4, D], F32)
            x1b = sb2("x1b", [128, 4, D], BF16)
            st1 = sb2("st1", [128, 4, 16], F32)
            st2 = sb2("st2", [128, 4, 16], F32)
            sl = sb2("sl", [128, 1, 512], F32)
            ln1g = sb2("ln1g", [128, D], F32)
            ln1b = sb2("ln1b", [128, D], F32)
            ln2g = sb2("ln2g", [128, D], F32)
            ln2b = sb2("ln2b", [128, D], F32)
            y2 = y
            T = Tok
            t_wo, t_wd, t_tab2 = T("wo"), T("wd"), T("tab2")
            t_tab2b = T("tab2b")
            t_wgu = [T("wgu%d" % i) for i in range(3)]
            t_hT, t_x1T = T("hT"), T("x1T")
            t_xin = [T("xin0"), T("xin1")]
            t_y = [T("y%d" % i) for i in range(4)]
            t_x1 = [T("x1_%d" % i) for i in range(4)]
            t_x1b = [T("x1b%d" % i) for i in range(4)]
            t_st1 = T("st1")
            t_st2 = [T("st2_%d" % i) for i in range(4)]
            t_sl = [T("sl0")] * 2
            t_y2 = t_y

            for j in range(4):
                P.dma("sp", y[:, j, :], xs[j * 128:(j + 1) * 128, :], "xg0_%d" % j, writes=[t_y[j]])
            t_wo4 = [T("wo%d" % i) for i in range(4)]
            wo_v = wo_s.rearrange("kc p n -> p kc n")
            for i in range(4):
                P.dma("sp", wo[:, 2 * i:2 * i + 2, :], wo_v[:, 2 * i:2 * i + 2, :], "wo%d" % i, writes=[t_wo4[i]])
            P.dma("sp", ln1g[:], ln1g_d, "c2a", writes=[t_tab2])
            P.dma("sp", ln1b[:], ln1b_d, "c2b", writes=[t_tab2])
            P.dma("sp", ln2g[:], ln2g_d, "c2c", writes=[t_tab2b])
            P.dma("sp", ln2b[:], ln2b_d, "c2d", writes=[t_tab2b])
            wd_v = wd_s.rearrange("fc p n -> p fc n")
            EPS1 = LN_EPS / (ALPHA * ALPHA)

            def ln_chain(views, vtoks, stt, t_stt, cols, g_t, b_t, post):
                for v, tk, c in zip(views, vtoks, cols):
                    P.op("dve", lambda e, v=v, c=c: e.bn_stats(stt[:, c, 0:6], v[:, 0:512]),
                         reads=[tk], writes=[t_stt])
                    P.op("dve", lambda e, v=v, c=c: e.bn_stats(stt[:, c, 6:12], v[:, 512:1024]),
                         reads=[tk], writes=[t_stt])
                    P.op("dve", lambda e, c=c: e.bn_aggr(stt[:, c, 12:14], stt[:, c, 0:12]),
                         reads=[t_stt], writes=[t_stt])
                return lambda: ln_part2(views, vtoks, stt, t_stt, cols, g_t, b_t, post)

            def ln_part2(views, vtoks, stt, t_stt, cols, g_t, b_t, post):
                c0, c1 = cols[0], cols[-1] + 1
                P.op("act", lambda e: e.activation(stt[:, c0:c1, 14], stt[:, c0:c1, 13], AF.Sqrt, bias=epsc[:, 2:3]),
                     reads=[t_stt, t_eps], writes=[t_stt])
                P.op("dve", lambda e: e.reciprocal(stt[:, c0:c1, 14], stt[:, c0:c1, 14]),
                     reads=[t_stt], writes=[t_stt])
                P.op("dve", lambda e: e.scalar_tensor_tensor(
                    stt[:, c0:c1, 15], stt[:, c0:c1, 12], -1.0, stt[:, c0:c1, 14], ALU.mult, ALU.mult),
                    reads=[t_stt], writes=[t_stt])
                for i, (v, tk, c) in enumerate(zip(views, vtoks, cols)):
                    P.op("act", lambda e, v=v, c=c: e.activation(
                        v, v, AF.Identity, scale=stt[:, c, 14:15], bias=stt[:, c, 15:16]),
                        reads=[t_stt, tk], writes=[tk])
                    P.op("dve", lambda e, v=v: e.tensor_tensor(v, v, g_t[:], ALU.mult),
                         reads=[tk, t_tab2], writes=[tk])
                    P.op("dve", lambda e, v=v: e.tensor_tensor(v, v, b_t[:], ALU.add),
                         reads=[tk, t_tab2], writes=[tk])
                    post(i)

            def W_mm(g, j):
                tt = g * 4 + j
                s = tt % 2
                if g == 0:
                    xres, t_xres = y[:, j, :], t_y[j]
                else:
                    P.dma("sp", xin[:, 0, :], xs[tt * 128:(tt + 1) * 128, :], "xin0", writes=[t_xin[0]])
                    xres, t_xres = xin[:, 0, :], t_xin[0]
                for hf in range(2):
                    bk = s * 2 + hf
                    for kc in range(8):
                        lhs = agT[:, kc, tt * 128:(tt + 1) * 128] if kc < 4 else sgT[:, kc - 4, tt * 128:(tt + 1) * 128]
                        P.op("pe", lambda e, lhs=lhs, bk=bk, kc=kc, hf=hf: e.matmul(
                            B[bk][:], lhs, wo[:, kc, hf * 512:(hf + 1) * 512], start=(kc == 0), stop=(kc == 7)),
                            reads=[t_agT, t_sgT, t_wo4[kc // 2]], writes=[tB[bk]])
                    P.op("dve", lambda e, bk=bk, hf=hf: e.scalar_tensor_tensor(
                        x1[:, j, hf * 512:(hf + 1) * 512], B[bk][:], 1.0 / ALPHA, xres[:, hf * 512:(hf + 1) * 512],
                        ALU.mult, ALU.add),
                        reads=[tB[bk], t_xres], writes=[t_x1[j]])

            def W_chain(g, js):
                def post(i):
                    j = js[i]
                    P.op("act", lambda e: e.copy(x1b[:, j, :], x1[:, j, :]),
                         reads=[t_x1[j]], writes=[t_x1b[j]])
                return ln_chain([x1[:, j, :] for j in js], [t_x1[j] for j in js], st1, t_st1, list(js),
                                ln1g, ln1b, post)

            def W_tr(g, j):
                bank, tbk = (B6, tB6) if j % 2 == 0 else (B7, tB7)
                for kc in range(8):
                    P.op("pe", lambda e, kc=kc: e.transpose(
                        bank[:, kc * 128:(kc + 1) * 128], x1b[:, j, kc * 128:(kc + 1) * 128], ident[:]),
                        reads=[t_x1b[j], t_ident], writes=[tbk])
                P.op("act", lambda e: e.copy(
                    x1T[:, :, j * 128:(j + 1) * 128], bank[:].rearrange("p (c k) -> p c k", k=128)),
                    reads=[tbk], writes=[t_x1T])

            def G_stage(g, steps=()):
                steps = list(steps)
                for fc in range(NFC):
                    ws_ = (g * NFC + fc) % 3
                    P.dma("sp", wg[:, ws_, :, :], wg_s[fc], "wgu%d" % ws_, writes=[t_wgu[ws_]])
                    P.dma("sp", wu[:, ws_, :, :], wu_s[fc], "wgu%d" % ws_, writes=[t_wgu[ws_]])
                    if g == 0 and 2 <= fc < 13:
                        i = fc - 2
                        P.dma("sp", wd[:, i * 2:(i + 1) * 2, :], wd_v[:, i * 2:(i + 1) * 2, :], "wd",
                              writes=[t_wd])
                    for kc in range(8):
                        P.op("pe", lambda e, kc=kc, ws_=ws_: e.matmul(
                            B[4][:], wg[:, ws_, kc, :], x1T[:, kc, :], start=(kc == 0), stop=(kc == 7)),
                            reads=[t_wgu[ws_], t_x1T], writes=[tB[4]])
                    for kc in range(8):
                        P.op("pe", lambda e, kc=kc, ws_=ws_: e.matmul(
                            B[5][:], wu[:, ws_, kc, :], x1T[:, kc, :], start=(kc == 0), stop=(kc == 7)),
                            reads=[t_wgu[ws_], t_x1T], writes=[tB[5]])
                    P.op("act", lambda e: e.activation(sl[:, 0, :], B[4][:], AF.Silu),
                         reads=[tB[4]], writes=[t_sl[0]])
                    P.op("dve", lambda e, fc=fc: e.tensor_tensor(hT[:, fc, :], sl[:, 0, :], B[5][:], ALU.mult),
                         reads=[tB[5], t_sl[0]], writes=[t_hT])
                    for _ in range(2):
                        if steps:
                            steps.pop(0)()
                while steps:
                    steps.pop(0)()

            def Dn(g, j):
                tt = g * 4 + j
                s = tt % 2
                for hf in range(2):
                    bk = s * 2 + hf
                    for fc in range(NFC):
                        P.op("pe", lambda e, bk=bk, fc=fc, hf=hf: e.matmul(
                            B[bk][:], hT[:, fc, j * 128:(j + 1) * 128], wd[:, fc, hf * 512:(hf + 1) * 512],
                            start=(fc == 0), stop=(fc == NFC - 1)),
                            reads=[t_hT, t_wd], writes=[tB[bk]])
                    P.op("dve", lambda e, bk=bk, hf=hf: e.scalar_tensor_tensor(
                        y[:, j, hf * 512:(hf + 1) * 512], B[bk][:], 1.0 / ALPHA, x1[:, j, hf * 512:(hf + 1) * 512],
                        ALU.mult, ALU.add),
                        reads=[tB[bk], t_x1[j]], writes=[t_y[j]])
                v, tk, stt, tst, c = y[:, j, :], t_y[j], st2, t_st2[j], j
                steps = []
                steps.append(lambda: P.op("dve", lambda e: e.bn_stats(stt[:, c, 0:6], v[:, 0:512]),
                                          reads=[tk], writes=[tst]))
                steps.append(lambda: (
                    P.op("dve", lambda e: e.bn_stats(stt[:, c, 6:12], v[:, 512:1024]), reads=[tk], writes=[tst]),
                    P.op("dve", lambda e: e.bn_aggr(stt[:, c, 12:14], stt[:, c, 0:12]), reads=[tst], writes=[tst])))
                steps.append(lambda: P.op(
                    "act", lambda e: e.activation(stt[:, c, 14:15], stt[:, c, 13:14], AF.Sqrt, bias=epsc[:, 2:3]),
                    reads=[tst, t_eps], writes=[tst]))
                steps.append(lambda: (
                    P.op("dve", lambda e: e.reciprocal(stt[:, c, 14:15], stt[:, c, 14:15]), reads=[tst], writes=[tst]),
                    P.op("dve", lambda e: e.scalar_tensor_tensor(
                        stt[:, c, 15:16], stt[:, c, 12:13], -1.0, stt[:, c, 14:15], ALU.mult, ALU.mult),
                        reads=[tst], writes=[tst])))
                steps.append(lambda: P.op(
                    "act", lambda e: e.activation(v, v, AF.Identity, scale=stt[:, c, 14:15], bias=stt[:, c, 15:16]),
                    reads=[tst, tk], writes=[tk]))
                steps.append(lambda: P.op("dve", lambda e: e.tensor_tensor(v, v, ln2g[:], ALU.mult),
                                          reads=[tk, t_tab2b], writes=[tk]))
                steps.append(lambda: (
                    P.op("dve", lambda e: e.tensor_tensor(v, v, ln2b[:], ALU.add), reads=[tk, t_tab2b], writes=[tk]),
                    P.dma("pool", out_d[tt * 128:(tt + 1) * 128, :], v, "out", reads=[tk])))
                return steps

            def round_robin(step_lists):
                out = []
                n = max(len(x) for x in step_lists)
                for i in range(n):
                    for x in step_lists:
                        if i < len(x):
                            out.append(x[i])
                return out

            for _ in range(24):
                P.op("pe", lambda e: e.matmul(B[5][:], ident[:], agT[:, 0, 0:512], start=True, stop=True),
                     reads=[t_ident, t_agT], writes=[tB[5]])
            W_mm(0, 0)
            c0 = W_chain(0, (0,))
            W_mm(0, 1)
            c0()
            c1 = W_chain(0, (1,))
            W_mm(0, 2)
            c1()
            c2 = W_chain(0, (2,))
            W_tr(0, 0)
            W_mm(0, 3)
            c2()
            c3 = W_chain(0, (3,))
            W_tr(0, 1)
            c3()
            W_tr(0, 2)
            W_tr(0, 3)
            pend = []
            for g in range(4):
                G_stage(g, pend)
                pend = []
                d0 = Dn(g, 0)
                d1 = Dn(g, 1)
                if g + 1 < 4:
                    W_mm(g + 1, 0)
                    W_mm(g + 1, 1)
                    cA = W_chain(g + 1, (0, 1))
                    d2 = Dn(g, 2)
                    cA()
                    W_mm(g + 1, 2)
                    c2 = W_chain(g + 1, (2,))
                    d3 = Dn(g, 3)
                    W_tr(g + 1, 0)
                    W_tr(g + 1, 1)
                    c2()
                    W_mm(g + 1, 3)
                    c3 = W_chain(g + 1, (3,))
                    W_tr(g + 1, 2)
                    c3()
                    W_tr(g + 1, 3)
                    pend = round_robin([d0, d1, d2, d3])
                else:
                    d2 = Dn(g, 2)
                    for st_ in round_robin([d0, d1]):
                        st_()
                    d3 = Dn(g, 3)
                    for st_ in round_robin([d2, d3]):
                        st_()
            P.emit()
    return nc


def _host_tables(e):
    own = A_BLK if e == 0 else B_BLK
    oth = B_BLK if e == 0 else A_BLK
    blocks = own + oth
    tok_idx = np.concatenate([np.arange(g * 256, (g + 1) * 256) for g in blocks])
    inv_freq = (10000.0 ** (-np.arange(0, 64, 2, dtype=np.float32) / 64.0)).astype(np.float32)
    ang = tok_idx.astype(np.float32)[:, None] * inv_freq[None, :]
    cos = np.cos(ang).astype(np.float32)
    sin = np.sin(ang).astype(np.float32)
    cs = np.concatenate([cos, -sin, sin], axis=1)
    csT = np.ascontiguousarray(cs.reshape(NT, 128, 96).transpose(1, 0, 2))
    eligb = np.zeros((16, 16), np.float32)
    elig01 = np.zeros((16, 16), np.float32)
    own01 = np.zeros((16, 16), np.float32)
    for tt in range(16):
        i = tt // 2
        gi = own[i]
        for kb in range(16):
            el = blocks[kb] < gi
            eligb[tt, kb] = 0.0 if el else -1e30
            elig01[tt, kb] = 1.0 if el else 0.0
            own01[tt, kb] = 1.0 if kb == i else 0.0
    bc = lambda a: np.ascontiguousarray(np.broadcast_to(a[None], (128,) + a.shape))
    return tok_idx, csT, bc(eligb), bc(elig01), bc(own01)


def _const_tables():
    onehot = np.zeros((16, S), np.float32)
    for kb in range(16):
        onehot[kb, kb * 256:(kb + 1) * 256] = 1.0
    cmask = np.zeros((128, 2, 256), np.float32)
    p = np.arange(128)[:, None]
    qq = np.arange(256)[None, :]
    for kt in range(2):
        cmask[:, kt, :] = np.where(kt * 128 + p <= qq, 0.0, NEG)
    ident = np.eye(128, dtype=np.float32)
    j = np.arange(128)[:, None]
    i = np.arange(128)[None, :]
    tril = (j <= i).astype(np.float32)
    bf = ml_dtypes.bfloat16
    return onehot.astype(bf), cmask.astype(bf), ident.astype(bf), tril.astype(bf)


_NC_CACHE = {}


def kernel(x, w_in, attn_out_g, gmlp_out_g, gmlp_ln_g, gmlp_ln_b, w_spatial, b_spatial,
           w_out, ln1_g, ln1_b, w_gate, w_up, w_down, ln2_g, ln2_b):
    f = lambda a: np.ascontiguousarray(np.asarray(a, dtype=np.float32))
    x = f(x)
    bc = lambda v, n: np.ascontiguousarray(np.broadcast_to(f(v).reshape(1, n), (128, n)))
    onehot, cmask, ident, tril = _const_tables()
    shared = {
        "w_in": f(w_in[0]), "w_out": f(w_out[0]), "w_gate": f(w_gate[0]), "w_up": f(w_up[0]),
        "w_down": f(w_down[0]),
        "onehot": onehot, "cmask": cmask, "ident": ident, "tril": tril,
        "wsT": np.ascontiguousarray(f(w_spatial[0]).transpose(2, 0, 1)),
        "bsT": np.ascontiguousarray(f(b_spatial[0]).T),
        "lng_b": bc(gmlp_ln_g[0], 512), "lnb_b": bc(gmlp_ln_b[0], 512), "gog_b": bc(gmlp_out_g[0], 512),
        "ag_fm": np.ascontiguousarray(f(attn_out_g[0]).reshape(4, 128).T),
        "ln1g_b": bc(ln1_g[0], D), "ln1b_b": bc(ln1_b[0], D),
        "ln2g_b": bc(ln2_g[0], D), "ln2b_b": bc(ln2_b[0], D),
    }
    in_maps = []
    idxs = []
    for c in range(8):
        b, e = c // 2, c % 2
        tok_idx, csT, eligb, elig01, own01 = _host_tables(e)
        idxs.append(tok_idx)
        m = dict(shared)
        m.update({"xs": np.ascontiguousarray(x[b][tok_idx]), "csT": csT,
                  "eligb": eligb, "elig01": elig01, "own01": own01})
        in_maps.append(m)
    if "nc" not in _NC_CACHE:
        _NC_CACHE["nc"] = build_program()
    nc = _NC_CACHE["nc"]
    res = run_bass_kernel_spmd(nc, in_maps, core_ids=list(range(8)))
    out = np.empty((4, S, D), np.float32)
    for c in range(8):
        b = c // 2
        out[b][idxs[c][:2048]] = res.results[c]["out"]
    return out
```
